# Optimizing a Trainium2 kernel written in Bass

```python
import jax
import jax.numpy as jnp
from jax import lax
import numpy as np

D_MODEL = 1024
BATCH = 4
SEQ = 4096
DEPTH = 2

N_MIXERS = 4
GROUP_WIDTH = D_MODEL // N_MIXERS
N_HEADS = 4
HEAD_DIM = GROUP_WIDTH // N_HEADS
CHUNK = 64
CONV_WIDTH = 4
GLA_RANK = 16
GLA_TAU = 16.0
ROPE_BASE = 10000.0
N_GROUPS = 4
EXPERTS_PER_GROUP = 4
N_EXPERTS = N_GROUPS * EXPERTS_PER_GROUP
TOP_K = 2
D_EXPERT = D_MODEL // 2
ALPHA = (2 * DEPTH) ** 0.25
BETA = (8 * DEPTH) ** -0.25
LN_EPS = 1e-5
NORM_EPS = 1e-6
W = GROUP_WIDTH
IN_SPLITS = (W, W, W, W, W, W, W, W, N_HEADS, N_HEADS, W, W, W, W, GLA_RANK, W, W, W, W)
IN_DIM = 16 * W + 2 * N_HEADS + GLA_RANK

kernel_name = "hybrid_parallel_mixer_moe_block"


def layer_norm(x, g, b):
    xf = x.astype(jnp.float32)
    mu = xf.mean(-1, keepdims=True)
    var = jnp.mean(jnp.square(xf - mu), axis=-1, keepdims=True)
    return ((xf - mu) * lax.rsqrt(var + LN_EPS)).astype(x.dtype) * g + b


def head_norm(x, gain, center):
    B, S, H, Dh = x.shape
    xf = x.astype(jnp.float32)
    if center:
        xf = xf - xf.mean(-1, keepdims=True)
    y = xf * lax.rsqrt(jnp.mean(xf * xf, axis=-1, keepdims=True) + NORM_EPS)
    return y.reshape(B, S, H * Dh).astype(x.dtype) * gain


def to_heads(t):
    return t.reshape(t.shape[0], t.shape[1], N_HEADS, -1)


def to_chunks(t):
    B, S, H, D = t.shape
    return t.reshape(B, S // CHUNK, CHUNK, H, D).transpose(1, 0, 3, 2, 4)


def from_chunks(t):
    N, B, H, C, D = t.shape
    return t.transpose(1, 0, 3, 2, 4).reshape(B, N * C, H, D)


def gate_chunks(t):
    B, S, H = t.shape
    return t.reshape(B, S // CHUNK, CHUNK, H).transpose(1, 0, 3, 2)


def rotary(x):
    S, Dh = x.shape[1], x.shape[-1]
    inv = ROPE_BASE ** (-jnp.arange(0, Dh, 2, dtype=jnp.float32) / Dh)
    ang = jnp.arange(S, dtype=jnp.float32)[:, None] * inv[None, :]
    cos = jnp.cos(ang)[None, :, None, :].astype(x.dtype)
    sin = jnp.sin(ang)[None, :, None, :].astype(x.dtype)
    x1, x2 = x[..., : Dh // 2], x[..., Dh // 2:]
    return jnp.concatenate([x1 * cos - x2 * sin, x1 * sin + x2 * cos], axis=-1)


def causal_conv(x, w):
    K = w.shape[0]
    return lax.conv_general_dilated(
        x, w[:, None, :].astype(x.dtype), window_strides=(1,), padding=[(K - 1, 0)],
        dimension_numbers=("NWC", "WIO", "NWC"), feature_group_count=x.shape[-1])


def retention_chunkwise(q, k, v):
    dt = v.dtype
    qc, kc, vc = (to_chunks(t.astype(jnp.float32)) for t in (q, k, v))
    _, B, H, C, Dk = qc.shape
    Dv = vc.shape[-1]
    log_gamma = jnp.log1p(-jnp.exp2(-5.0 - jnp.arange(H, dtype=jnp.float32)))
    pos = jnp.arange(1, C + 1, dtype=jnp.float32)
    rel = pos[:, None] - pos[None, :]
    decay_intra = jnp.where(rel >= 0, jnp.exp(log_gamma[:, None, None] * jnp.maximum(rel, 0.0)), 0.0)
    decay_q = jnp.exp(log_gamma[:, None] * pos[None, :])[:, :, None]
    decay_k = jnp.exp(log_gamma[:, None] * (C - pos)[None, :])[:, :, None]
    decay_chunk = jnp.exp(log_gamma * C)[:, None, None]

    def step(state, inp):
        qi, ki, vi = inp
        scores = jnp.einsum("bhid,bhjd->bhij", qi, ki) * decay_intra
        out = jnp.einsum("bhij,bhjv->bhiv", scores, vi) + jnp.einsum("bhid,bhdv->bhiv", qi, state) * decay_q
        state = state * decay_chunk + jnp.einsum("bhjd,bhjv->bhdv", ki * decay_k, vi)
        return state, out

    init = jnp.zeros((B, H, Dk, Dv), jnp.float32)
    _, out = lax.scan(step, init, (qc, kc, vc))
    return from_chunks(out).astype(dt)


def mlstm_chunkwise(q, k, v, i_pre, log_f):
    dt = v.dtype
    qc, kc, vc = (to_chunks(t.astype(jnp.float32)) for t in (q, k, v))
    ic, fc = (gate_chunks(t.astype(jnp.float32)) for t in (i_pre, log_f))
    _, B, H, C, Dk = qc.shape
    Dv = vc.shape[-1]
    causal = jnp.tril(jnp.ones((C, C), dtype=bool))

    def step(carry, inp):
        mem, nvec, m_prev = carry
        qi, ki, vi, ii, fi = inp
        b = jnp.cumsum(fi, axis=-1)
        log_w = jnp.where(causal, b[..., :, None] - b[..., None, :] + ii[..., None, :], -jnp.inf)
        log_inter = b + m_prev[..., None]
        m = jnp.maximum(log_inter, log_w.max(-1))
        s = jnp.einsum("bhid,bhjd->bhij", qi, ki) * jnp.exp(log_w - m[..., None])
        w_inter = jnp.exp(log_inter - m)
        num = jnp.einsum("bhij,bhjv->bhiv", s, vi) + w_inter[..., None] * jnp.einsum("bhid,bhdv->bhiv", qi, mem)
        den = s.sum(-1) + w_inter * jnp.einsum("bhid,bhd->bhi", qi, nvec)
        h = num / jnp.maximum(jnp.abs(den), jnp.exp(-m))[..., None]
        log_end_inter = b[..., -1] + m_prev
        log_end = b[..., -1:] - b + ii
        m_new = jnp.maximum(log_end_inter, log_end.max(-1))
        wk = jnp.exp(log_end - m_new[..., None])
        decay = jnp.exp(log_end_inter - m_new)
        mem = decay[..., None, None] * mem + jnp.einsum("bhjd,bhjv->bhdv", ki * wk[..., None], vi)
        nvec = decay[..., None] * nvec + jnp.einsum("bhj,bhjd->bhd", wk, ki)
        return (mem, nvec, m_new), h

    init = (jnp.zeros((B, H, Dk, Dv), jnp.float32), jnp.zeros((B, H, Dk), jnp.float32),
            jnp.zeros((B, H), jnp.float32))
    _, out = lax.scan(step, init, (qc, kc, vc, ic, fc))
    return from_chunks(out).astype(dt)


def chunked_gated_linear_attention(q, k, v, log_f):
    dt = v.dtype
    qc, kc, vc, gc = (to_chunks(t.astype(jnp.float32)) for t in (q, k, v, log_f))
    _, B, H, C, Dk = qc.shape
    Dv = vc.shape[-1]
    causal = jnp.tril(jnp.ones((C, C), dtype=bool))[:, :, None]

    def step(state, inp):
        qi, ki, vi, gi = inp
        G = jnp.cumsum(gi, axis=2)
        w = jnp.exp(jnp.where(causal, G[:, :, :, None, :] - G[:, :, None, :, :], -jnp.inf))
        scores = jnp.einsum("bhid,bhjd,bhijd->bhij", qi, ki, w)
        out = jnp.einsum("bhij,bhjv->bhiv", scores, vi) + jnp.einsum("bhid,bhdv->bhiv", qi * jnp.exp(G), state)
        G_end = G[:, :, -1:, :]
        state = state * jnp.exp(G_end)[:, :, 0, :, None] + jnp.einsum("bhjd,bhjv->bhdv", ki * jnp.exp(G_end - G), vi)
        return state, out

    init = jnp.zeros((B, H, Dk, Dv), jnp.float32)
    _, out = lax.scan(step, init, (qc, kc, vc, gc))
    return from_chunks(out).astype(dt)


def hybrid_mixer(h, w_in, conv_w, gate_b, gla_w2, gla_b2, lb, ret_g, mlstm_g, gla_g, hgrn_g, w_out):
    proj = h @ w_in
    (r_q, r_k, r_v, r_g,
     m_q, m_k, m_v, m_o, m_i, m_f,
     g_q, g_k, g_v, g_g, g_lr,
     h_q, h_f, h_i, h_g) = jnp.split(proj, np.cumsum(IN_SPLITS)[:-1].tolist(), axis=-1)
    scale = HEAD_DIM ** -0.5

    ro = retention_chunkwise(rotary(to_heads(r_q)) * scale, rotary(to_heads(r_k)), to_heads(r_v))
    y_ret = head_norm(ro, ret_g, True) * jax.nn.silu(r_g)

    qk = jax.nn.silu(causal_conv(jnp.concatenate([m_q, m_k], axis=-1), conv_w))
    mq, mk = jnp.split(qk, 2, axis=-1)
    i_pre = m_i + gate_b[:N_HEADS]
    log_fm = jax.nn.log_sigmoid(m_f + gate_b[N_HEADS:])
    mh = mlstm_chunkwise(to_heads(mq), to_heads(mk) * scale, to_heads(m_v), i_pre, log_fm)
    y_mlstm = head_norm(mh * to_heads(jax.nn.sigmoid(m_o)), mlstm_g, True)

    log_a = jax.nn.log_sigmoid(g_lr @ gla_w2 + gla_b2) / GLA_TAU
    go = chunked_gated_linear_attention(to_heads(g_q), to_heads(g_k) * scale, to_heads(g_v), to_heads(log_a))
    y_gla = head_norm(go, gla_g, False) * jax.nn.silu(g_g)

    z = h_f.astype(jnp.float32)
    log_fh = jnp.logaddexp(jnp.log(lb), jnp.log1p(-lb) + jax.nn.log_sigmoid(z))
    k_h = (1.0 - lb) * jax.nn.sigmoid(-z)
    ho = chunked_gated_linear_attention(to_heads(jax.nn.silu(h_q)), to_heads(k_h), to_heads(h_i), to_heads(log_fh))
    y_hgrn = head_norm(ho * to_heads(jax.nn.sigmoid(h_g)), hgrn_g, False)

    return jnp.concatenate([y_ret, y_mlstm, y_gla, y_hgrn], axis=-1) @ w_out


def group_limited_moe(h, router_w, router_b, w1, w3, w2):
    B, S, D = h.shape
    T = B * S
    ht = h.reshape(T, D)
    probs = jax.nn.softmax((ht @ router_w).astype(jnp.float32), axis=-1)
    sel = (probs + router_b.astype(jnp.float32)).reshape(T, N_GROUPS, EXPERTS_PER_GROUP)
    group_score = lax.top_k(sel, TOP_K)[0].sum(-1)
    in_group = jax.nn.one_hot(jnp.argmax(group_score, axis=-1), N_GROUPS, dtype=bool)
    masked = jnp.where(in_group[:, :, None], sel, -jnp.inf).reshape(T, N_EXPERTS)
    _, idx = lax.top_k(masked, TOP_K)
    wts = jnp.take_along_axis(probs, idx, axis=-1)
    wts = wts / wts.sum(-1, keepdims=True)
    gates = jnp.einsum("tk,tke->te", wts, jax.nn.one_hot(idx, N_EXPERTS, dtype=jnp.float32)).astype(h.dtype)
    y = jnp.zeros_like(ht)
    for e in range(N_EXPERTS):
        a = jax.nn.silu(ht @ w1[e]) * (ht @ w3[e])
        y = y + gates[:, e:e + 1] * (a @ w2[e])
    return y.reshape(B, S, D)


def setup_inputs(seed: int = 0) -> dict:
    key = jax.random.key(seed)
    ks = jax.random.split(key, 26)
    D = D_MODEL

    def nrm(k, shape, s):
        return jax.random.normal(k, shape, jnp.float32) * s

    gate_b = jnp.concatenate(
        [nrm(ks[6], (DEPTH, N_HEADS), 0.1),
         jnp.linspace(3.0, 6.0, N_HEADS, dtype=jnp.float32)[None, :] + nrm(ks[7], (DEPTH, N_HEADS), 0.1)], axis=-1)
    return {
        "x": nrm(ks[0], (BATCH, SEQ, D), 1.0),
        "c": nrm(ks[1], (BATCH, D), 1.0),
        "ada_w": nrm(ks[2], (DEPTH, D, 6 * D), 0.5 * D ** -0.5),
        "ada_b": nrm(ks[3], (DEPTH, 6 * D), 0.02),
        "w_in": nrm(ks[4], (DEPTH, D, IN_DIM), D ** -0.5),
        "mlstm_conv": nrm(ks[5], (DEPTH, CONV_WIDTH, 2 * W), CONV_WIDTH ** -0.5),
        "mlstm_gate_b": gate_b,
        "gla_w2": nrm(ks[8], (DEPTH, GLA_RANK, W), GLA_RANK ** -0.5),
        "gla_b2": nrm(ks[9], (DEPTH, W), 0.02),
        "hgrn_lb": nrm(ks[10], (DEPTH, W), 0.1),
        "ret_norm": 1.0 + nrm(ks[11], (DEPTH, W), 0.02),
        "mlstm_norm": 1.0 + nrm(ks[12], (DEPTH, W), 0.02),
        "gla_norm": 1.0 + nrm(ks[13], (DEPTH, W), 0.02),
        "hgrn_norm": 1.0 + nrm(ks[14], (DEPTH, W), 0.02),
        "w_out": nrm(ks[15], (DEPTH, D, D), BETA * D ** -0.5),
        "ln1_g": 1.0 + nrm(ks[16], (DEPTH, D), 0.02),
        "ln1_b": nrm(ks[17], (DEPTH, D), 0.02),
        "router_w": nrm(ks[18], (D, N_EXPERTS), D ** -0.5),
        "router_b": nrm(ks[19], (N_EXPERTS,), 0.01),
        "exp_w1": nrm(ks[20], (DEPTH, N_EXPERTS, D, D_EXPERT), D ** -0.5),
        "exp_w3": nrm(ks[21], (DEPTH, N_EXPERTS, D, D_EXPERT), D ** -0.5),
        "exp_w2": nrm(ks[22], (DEPTH, N_EXPERTS, D_EXPERT, D), BETA * D_EXPERT ** -0.5),
        "ln2_g": 1.0 + nrm(ks[23], (DEPTH, D), 0.02),
        "ln2_b": nrm(ks[24], (DEPTH, D), 0.02),
    }


def reference(x, c, ada_w, ada_b, w_in, mlstm_conv, mlstm_gate_b, gla_w2, gla_b2, hgrn_lb,
              ret_norm, mlstm_norm, gla_norm, hgrn_norm, w_out, ln1_g, ln1_b,
              router_w, router_b, exp_w1, exp_w3, exp_w2, ln2_g, ln2_b):
    lb_all = jnp.cumsum(jax.nn.softmax(hgrn_lb.astype(jnp.float32), axis=0), axis=0)
    lb_all = lb_all - lb_all[:1]
    cond = jax.nn.silu(c)
    for l in range(DEPTH):
        mod = (cond @ ada_w[l] + ada_b[l])[:, None, :]
        sh1, sc1, g1, sh2, sc2, g2 = jnp.split(mod, 6, axis=-1)
        h = x * (1 + sc1) + sh1
        mix = hybrid_mixer(h, w_in[l], mlstm_conv[l], mlstm_gate_b[l], gla_w2[l], gla_b2[l], lb_all[l],
                           ret_norm[l], mlstm_norm[l], gla_norm[l], hgrn_norm[l], w_out[l])
        x = layer_norm(ALPHA * x + g1 * mix, ln1_g[l], ln1_b[l])
        h = x * (1 + sc2) + sh2
        ffn = group_limited_moe(h, router_w, router_b, exp_w1[l], exp_w3[l], exp_w2[l])
        x = layer_norm(ALPHA * x + g2 * ffn, ln2_g[l], ln2_b[l])
    return x
```

```python
import math
from contextlib import ExitStack

import numpy as np
import ml_dtypes

import concourse.bass as bass
import concourse.mybir as mybir
from concourse.alu_op_type import AluOpType as ALU
from concourse.bass_utils import run_bass_kernel_spmd

F32 = mybir.dt.float32
BF16 = mybir.dt.bfloat16
AF = mybir.ActivationFunctionType
AX = mybir.AxisListType

D = 1024
SEQ = 4096
NB = 4
DEPTH = 2
NE = 16
DE = 512
ALPHA = (2 * DEPTH) ** 0.25
LN_EPS = 1e-5
NORM_EPS = 1e-6
NTILE_MIX = SEQ // 128
NOWN = SEQ // 2
NTILE_TOK = NOWN // 128
NPROJ = 2068
ENGS = ("pe", "dve", "act", "pool", "sp")

C_ID, C_LT, C_MASK, C_SEL = 0, 128, 256, 384
C_SH = 388
C_SP = C_SH + 3 * 128
C_CM = C_SP + 3 * 128
C_HM = C_CM + 8
NCONST = C_HM + 2


class Sched:
    def __init__(self, nc, es, n_dma_sems=16):
        self.nc = nc
        self.ops = {e: [] for e in ENGS}
        self.sem = {e: es.enter_context(nc.semaphore("s_" + e)) for e in ENGS}
        self.cnt = {e: 0 for e in ENGS}
        self.dma_sems = [[es.enter_context(nc.semaphore("d_%d" % i)), 0, None] for i in range(n_dma_sems)]
        self.cc_sems = []
        self.es = es
        self.dma_rr = 0
        self.res = {}
        self.waited = {e: {} for e in ENGS}
        self.n_inst = 0

    def _need(self, eng, tok, waits):
        if tok is None:
            return
        sem, val, src = tok
        if src == eng and eng == "pe":
            return
        key = id(sem)
        if self.waited[eng].get(key, 0) >= val:
            return
        if key in waits:
            val = max(val, waits[key][1])
        waits[key] = (sem, val)

    def op(self, eng, fn, reads=(), writes=(), dma=False, cc=False):
        import os as _os
        if self.n_inst >= int(_os.environ.get("MAXOPS", "100000000")):
            if not getattr(self, "_delayed", False) and _os.environ.get("DELAYN"):
                self._delayed = True
                for _i in range(int(_os.environ["DELAYN"])):
                    self.cnt["act"] += 1
                    self.ops["act"].append(([], self.delay_fn, (self.sem["act"], 1)))
            return None
        if str(self.n_inst) in _os.environ.get("SKIPOPS", "").split(","):
            self.n_inst += 1
            return None
        if _os.environ.get("OPTRACE"):
            import inspect
            fr = inspect.stack()
            ln = [f.lineno for f in fr[1:4]]
            print("OP", self.n_inst, eng, ln, "dma" if dma else "")
        waits = {}
        for k in reads:
            st = self.res.get(k)
            if st is not None:
                self._need(eng, st["w"], waits)
        for k in writes:
            st = self.res.get(k)
            if st is not None:
                self._need(eng, st["w"], waits)
                for t in st["r"]:
                    if t[2] != eng or dma or cc:
                        self._need(eng, t, waits)
        if cc:
            sem = self.es.enter_context(self.nc.semaphore("cc_%d" % len(self.cc_sems)))
            self.cc_sems.append(sem)
            tok = (sem, 1, "cc")
            inc = (sem, None)
        elif dma:
            slot = self.dma_sems[self.dma_rr]
            self.dma_rr = (self.dma_rr + 1) % len(self.dma_sems)
            self._need(eng, slot[2], waits)
            slot[1] += 16
            tok = (slot[0], slot[1], "dma")
            slot[2] = tok
            inc = (slot[0], 16)
        else:
            self.cnt[eng] += 1
            tok = (self.sem[eng], self.cnt[eng], eng)
            inc = (self.sem[eng], 1)
        wl = list(waits.values())
        for sem, val in wl:
            self.waited[eng][id(sem)] = val
        self.ops[eng].append((wl, fn, inc))
        for k in reads:
            st = self.res.setdefault(k, {"w": None, "r": []})
            st["r"].append(tok)
        for k in writes:
            self.res[k] = {"w": tok, "r": []}
        self.n_inst += 1
        return tok

    def barrier(self):
        toks = [(self.sem[e], self.cnt[e], e) for e in ENGS if self.cnt[e] > 0]
        toks += [(s[0], s[1], "dma") for s in self.dma_sems if s[1] > 0]
        toks += [(s, 1, "cc") for s in self.cc_sems]
        for e in ENGS:
            waits = {}
            for t in toks:
                if t[2] == e:
                    continue
                key = id(t[0])
                if self.waited[e].get(key, 0) >= t[1]:
                    continue
                waits[key] = (t[0], t[1])
                self.waited[e][key] = t[1]
            if waits:
                self.ops[e].append((list(waits.values()), None, None))
        self.res = {}

    def emit(self):
        nc = self.nc
        ops = self.ops
        self.ops = {e: [] for e in ENGS}
        with nc.Block() as block:
            def mk(ename):
                def body(e):
                    for waits, fn, inc in ops[ename]:
                        for sem, val in waits:
                            e.wait_ge(sem, val)
                        if fn is not None:
                            ins = fn(e)
                            if inc[1] is None:
                                ins.then_inc(inc[0])
                            else:
                                ins.then_inc(inc[0], inc[1])
                return body
            block.tensor(mk("pe"))
            block.vector(mk("dve"))
            block.scalar(mk("act"))
            block.gpsimd(mk("pool"))
            block.sync(mk("sp"))


class T:
    def __init__(self, h, F, dt):
        self.h, self.F, self.dt = h, F, dt

    def ap(self, off=0, dims=None, p0=0, n=128):
        if dims is None:
            dims = [[1, self.F - off]]
        return bass.AP(self.h, p0 * self.F + off, [[self.F, n]] + [list(d) for d in dims])

    def c(self, c0, c1, p0=0, n=128):
        return self.ap(c0, [[1, c1 - c0]], p0, n)


def dap(h, off, dims):
    return bass.AP(h, off, [list(d) for d in dims])


def build(layer=0, part="mix", dbg=None, n_cores=8, stop_after=None, mix_tiles=NTILE_MIX, layers=None):
    nc = bass.Bass("TRN2", target_bir_lowering=False)
    dbg = dbg or {}

    def din(name, shape, dt=F32):
        return nc.dram_tensor(name, list(shape), dt, kind="ExternalInput")

    layers = [layer] if layers is None else list(layers)
    nl = len(layers)
    fused = part == "fused"
    has_mix = part in ("mix", "fused")
    has_tok = part in ("tok", "fused")
    pairs = [[2 * i, 2 * i + 1] for i in range(n_cores // 2)]
    xfull = din("xfull", [SEQ, D]) if has_mix else None
    xown = din("xown", [NOWN, D])
    cT_d = din("cT", [128, 8])
    ada_w = din("ada_w", [nl, D, 6 * D])
    ada_bT = din("ada_bT", [DEPTH, 128, 48])
    ada_bg = din("ada_bg", [DEPTH, 2, D])
    w_in_c = din("w_in_c", [DEPTH, D, NPROJ])
    conv_c = din("conv_c", [DEPTH, 4, 256])
    gateb_c = din("gateb_c", [DEPTH, 4])
    glaw2_c = din("glaw2_c", [DEPTH, 17, 128])
    hlb_c = din("hlb_c", [DEPTH, 128])
    gain_c = din("gain_c", [DEPTH, 512])
    w_out_p = din("w_out_p", [DEPTH, D, D])
    ln1_g = din("ln1_g", [DEPTH, D])
    ln1_b = din("ln1_b", [DEPTH, D])
    ln2_g = din("ln2_g", [DEPTH, D])
    ln2_b = din("ln2_b", [DEPTH, D])
    router_w = din("router_w", [D, NE])
    router_b = din("router_b", [1, NE])
    if has_tok:
        exp_w1 = din("exp_w1", [nl, NE, D, DE])
        exp_w3 = din("exp_w3", [nl, NE, D, DE])
        exp_w2 = din("exp_w2", [nl, NE, DE, D])
    consts_d = din("consts", [128, NCONST])
    lgam_d = din("lgam", [128, 128])
    rot_d = din("rot", [SEQ, 64])
    sel_d = din("sel", [128, 2])
    if part == "mix":
        yout_d = nc.dram_tensor("yout", [SEQ, 512], BF16, kind="ExternalOutput")
    if part == "tok":
        ygath_d = din("ygath", [2 * SEQ, 512], BF16)
    if has_tok:
        out_d = nc.dram_tensor("out", [NOWN, D], F32, kind="ExternalOutput")
    if fused:
        yb_in = [[nc.dram_tensor("yb_in%d_%d" % (i, h), [NOWN, 512], BF16) for h in range(2)] for i in range(nl)]
        yb_out = [[nc.dram_tensor("yb_out%d_%d" % (i, h), [2 * NOWN, 512], BF16) for h in range(2)] for i in range(nl)]
        xg_in = [nc.dram_tensor("xg_in%d" % q, [512, D], F32) for q in range(4)]
        xg_out = [nc.dram_tensor("xg_out%d" % q, [1024, D], F32) for q in range(4)]
    dbg_out = {k: nc.dram_tensor("dbg_" + k, list(v), F32, kind="ExternalOutput") for k, v in dbg.items()}


    with ExitStack() as es:
        S = Sched(nc, es)

        uniq = [0]

        def sb(name, F, dt=F32, stack=es):
            uniq[0] += 1
            return T(stack.enter_context(nc.sbuf_tensor("sb%d_%s" % (uniq[0], name), [128, F], dt)), F, dt)

        def DMA(eng, out, in_, reads=(), writes=()):
            S.op(eng, lambda e, o=out, i=in_: e.dma_start(out=o, in_=i), reads, writes, dma=True)

        def MM(out, lhsT, rhs, reads, writes, start=True, stop=True, tp=None):
            S.op("pe", lambda e, o=out, l=lhsT, r=rhs, a=start, b=stop, t=tp:
                 e.matmul(o, l, r, start=a, stop=b, tile_position=t), reads, writes)

        def TR(out, in_, ident, reads, writes):
            S.op("pe", lambda e, o=out, i=in_, d=ident: e.transpose(o, i, d), reads, writes)

        def ACT(out, in_, func, reads, writes, scale=None, bias=None):
            kw = {}
            if scale is not None:
                kw["scale"] = scale
            if bias is not None:
                kw["bias"] = bias
            S.op("act", lambda e, o=out, i=in_, f=func, k=kw: e.activation(o, i, f, **k), reads, writes)

        def TT(eng, out, in0, in1, op, reads, writes):
            S.op(eng, lambda e, o=out, a=in0, b=in1, p=op: e.tensor_tensor(o, a, b, p), reads, writes)

        def TS(eng, out, in0, s1, s2, op0, op1, reads, writes):
            if op1 is None:
                S.op(eng, lambda e, o=out, a=in0, x=s1, p=op0: e.tensor_scalar(o, a, x, None, p), reads, writes)
            else:
                S.op(eng, lambda e, o=out, a=in0, x=s1, y=s2, p=op0, q=op1:
                     e.tensor_scalar(o, a, x, y, p, q), reads, writes)

        def STT(out, in0, scalar, in1, op0, op1, reads, writes):
            S.op("dve", lambda e, o=out, a=in0, s=scalar, b=in1, p=op0, q=op1:
                 e.scalar_tensor_tensor(o, a, s, b, p, q), reads, writes)

        def CP(eng, out, in_, reads, writes):
            if eng == "act":
                ACT(out, in_, AF.Copy, reads, writes)
            else:
                S.op(eng, lambda e, o=out, i=in_: e.tensor_copy(o, i), reads, writes)

        def RED(out, in_, op, reads, writes):
            S.op("dve", lambda e, o=out, i=in_, p=op: e.tensor_reduce(o, i, AX.X, p), reads, writes)

        def RECIP(out, in_, reads, writes):
            S.op("dve", lambda e, o=out, i=in_: e.reciprocal(o, i), reads, writes)

        def dump(name, src_ap, reads):
            if name in dbg_out:
                shp = dbg[name]
                dims = [[int(np.prod(shp[1:])), shp[0]]]
                if len(shp) == 2:
                    dims.append([1, shp[1]])
                else:
                    dims += [[int(np.prod(shp[i + 1:])), shp[i]] for i in range(1, len(shp))]
                DMA("sp", dap(dbg_out[name], 0, dims), src_ap, reads=reads)

        xres = sb("xres", NTILE_TOK * D)
        S.delay_fn = lambda e: e.activation(xres.c(15 * D, 16 * D), xres.c(15 * D, 16 * D), AF.Copy)
        cst = sb("cst", NCONST)
        cstb = sb("cstb", NCONST, BF16)
        sel = sb("sel", 2)
        ids0 = sb("ids0", 128, BF16)
        ids1 = sb("ids1", 128, BF16)
        ones1 = sb("ones1", 128)
        condT = sb("condT", 8)
        modT = sb("modT", 32)
        gbc1 = sb("gbc1", D)
        gbc2 = sb("gbc2", D)
        rwf = sb("rwf", 8 * NE)
        rbb = sb("rbb", NE)
        gates = sb("gates", NTILE_TOK * NE)
        wbig = sb("wbig", 2 * 12288, BF16)
        pb = [T(es.enter_context(nc.psum_tensor("pb%d" % i, [128, 512], F32)), 512, F32) for i in range(8)]
        pbh = [T(p.h.bitcast(BF16), 1024, BF16) for p in pb]

        ident = cst.c(C_ID, C_ID + 128)
        identb = cstb.c(C_ID, C_ID + 128)

        DMA("sp", cst.ap(), consts_d.ap(), writes=["cst"])
        DMA("sp", sel.ap(), sel_d.ap(), writes=["sel"])
        DMA("sp", condT.ap(), cT_d.ap(), writes=["condT"])
        DMA("sp", rwf.ap(0, [[NE, 8], [1, NE]]), dap(router_w, 0, [[NE, 128], [128 * NE, 8], [1, NE]]), writes=["rwf"])
        DMA("sp", rbb.ap(), dap(router_b, 0, [[0, 128], [1, NE]]), writes=["rbb"])
        for t in range(NTILE_TOK):
            DMA("sp" if t % 2 == 0 else "act", xres.c(t * D, (t + 1) * D),
                dap(xown, t * 128 * D, [[D, 128], [1, D]]), writes=["xres%d" % t])
        CP("dve", cstb.ap(), cst.ap(), ["cst"], ["cstb"])
        TS("dve", ids0.ap(), cst.c(C_ID, C_ID + 128), sel.c(0, 1), None, ALU.mult, None, ["cst", "sel"], ["ids0"])
        TS("dve", ids1.ap(), cst.c(C_ID, C_ID + 128), sel.c(1, 2), None, ALU.mult, None, ["cst", "sel"], ["ids1"])
        S.op("pool", lambda e: e.memset(ones1.ap(), 1.0), writes=["ones1"])
        ACT(condT.ap(), condT.ap(), AF.Silu, ["condT"], ["condT"])
        S.barrier()
        S.emit()

        for li, l in enumerate(layers):
            last = li == nl - 1
            if stop_after == "setup":
                break
            with ExitStack() as ph:
                stg = [sb("adastg%d" % i, 8 * 512, stack=ph) for i in range(2)]
                abT = sb("abT", 48, stack=ph)
                abg = sb("abg", 2 * D, stack=ph)
                grow = sb("grow", 2 * D, stack=ph)
                DMA("sp", abT.ap(), dap(ada_bT, l * 128 * 48, [[48, 128], [1, 48]]), writes=["abT"])
                DMA("sp", abg.ap(0, [[1, 2 * D]], 0, 1), dap(ada_bg, l * 2 * D, [[0, 1], [1, 2 * D]]), writes=["abg"])
                kind_of = {0: 1, 1: 0, 3: 3, 4: 2}
                for hc in range(12):
                    ch, half = hc // 2, hc % 2
                    st_ = stg[hc % 2]
                    k_st = "adastg%d" % (hc % 2)
                    DMA("sp" if hc % 2 == 0 else "act", st_.ap(0, [[512, 8], [1, 512]]),
                        dap(ada_w, li * D * 6 * D + hc * 512, [[6 * D, 128], [128 * 6 * D, 8], [1, 512]]),
                        writes=[k_st])
                    if ch in kind_of:
                        kind = kind_of[ch]
                        for j in range(4):
                            jj = half * 4 + j
                            for kc in range(8):
                                MM(pb[0].ap(jj, [[1, 1]]), st_.ap(kc * 512 + j * 128, [[1, 128]]),
                                   condT.c(kc, kc + 1), [k_st, "condT"], ["pb0"], start=(kc == 0), stop=(kc == 7))
                        if half == 1:
                            TT("dve", modT.c(kind * 8, kind * 8 + 8), pb[0].c(0, 8), abT.c(ch * 8, ch * 8 + 8),
                               ALU.add, ["pb0", "abT"], ["modT"])
                            if kind in (0, 2):
                                TS("dve", modT.c(kind * 8, kind * 8 + 8), modT.c(kind * 8, kind * 8 + 8), 1.0, None,
                                   ALU.add, None, ["modT"], ["modT"])
                    else:
                        gi = 0 if ch == 2 else 1
                        for kc in range(8):
                            MM(pb[1].ap(0, [[1, 512]], 0, 1), condT.c(kc, kc + 1), st_.ap(kc * 512, [[1, 512]]),
                               [k_st, "condT"], ["pb1"], start=(kc == 0), stop=(kc == 7))
                        TT("dve", grow.ap(gi * D + half * 512, [[1, 512]], 0, 1), pb[1].ap(0, [[1, 512]], 0, 1),
                           abg.ap(gi * D + half * 512, [[1, 512]], 0, 1), ALU.add, ["pb1", "abg"], ["grow"])
                TS("dve", grow.ap(D, [[1, D]], 0, 1), grow.ap(D, [[1, D]], 0, 1), 1.0 / ALPHA, None, ALU.mult, None,
                   ["grow"], ["grow"])
                for gi, gb_ in enumerate((gbc1, gbc2)):
                    for half in range(2):
                        MM(pb[2 + half].ap(), ones1.ap(0, [[1, 128]], 0, 1),
                           grow.ap(gi * D + half * 512, [[1, 512]], 0, 1), ["ones1", "grow"], ["pb%d" % (2 + half)])
                        CP("dve", gb_.c(half * 512, half * 512 + 512), pb[2 + half].ap(), ["pb%d" % (2 + half)],
                           ["gbc"])
                dump("modT%d" % l, modT.ap(), ["modT"])
                dump("gbc1_%d" % l, gbc1.ap(), ["gbc"])
                S.barrier()
                S.emit()
            if stop_after == "mod":
                break

            if not has_mix:
                pass
            else:
              with ExitStack() as ph:
                w_in = T(wbig.h, wbig.F, BF16)
                WIN = NPROJ
                xt = [sb("xt%d" % i, D, stack=ph) for i in range(2)]
                rot = [sb("rot%d" % i, 64, stack=ph) for i in range(2)]
                hT = [sb("hT%d" % i, 8 * 128, BF16, stack=ph) for i in range(2)]
                P = sb("P", NPROJ, stack=ph)
                rq = sb("rq", 256, stack=ph)
                rtmp = sb("rtmp", 4 * 128, stack=ph)
                qf = sb("qf", 512, stack=ph)
                kf = sb("kf", 512, stack=ph)
                vx = sb("vx", 8 * 65 + 8, BF16, stack=ph)
                gfull = sb("gfull", 512, stack=ph)
                eG = sb("eG", 512, stack=ph)
                enG = sb("enG", 512, stack=ph)
                Qt = sb("Qt", 512, BF16, stack=ph)
                Kt = sb("Kt", 512, BF16, stack=ph)
                QT = sb("QT", 512, BF16, stack=ph)
                QTb = sb("QTb", 512, BF16, stack=ph)
                KT = sb("KT", 512, BF16, stack=ph)
                Sm = sb("Sm", 8 * 128, BF16, stack=ph)
                Sraw = sb("Sraw", 8 * 128, BF16, stack=ph)
                acT = sb("acT", 16, stack=ph)
                state = sb("state", 4 * 65, stack=ph)
                stmp = sb("stmp", 4 * 65, stack=ph)
                stb = sb("stb", 4 * 4 * 65 + 8, BF16, stack=ph)
                xs = [sb("xs%d" % i, 4 * 256, BF16, stack=ph) for i in range(2)]
                qkm = sb("qkm", 256, stack=ph)
                mqk = sb("mqk", 256, stack=ph)
                sm = sb("sm", 20, stack=ph)
                gsm = sb("gsm", 16, stack=ph)
                lrT = sb("lrT", 128, stack=ph)
                glaw = sb("glaw", 128, stack=ph)
                gtmp = sb("gtmp", 128, stack=ph)
                htmp = sb("htmp", 128, stack=ph)
                hf = sb("hf", 128, stack=ph)
                U = sb("U", 512, stack=ph)
                Ob = sb("Ob", 520, stack=ph)
                Uc = sb("Uc", 512, stack=ph)
                sq = sb("sq", 512, stack=ph)
                gg = sb("gg", 512, stack=ph)
                nsm = sb("nsm", 40, stack=ph)
                Y = [sb("Y%d" % i, 512, BF16, stack=ph) for i in range(2)]
                gainb = sb("gainb", 512, stack=ph)
                convb = sb("convb", 4 * 256, stack=ph)
                gbb = sb("gbb", 4, stack=ph)
                lb0 = sb("lb0", 128, stack=ph)
                lb1 = sb("lb1", 128, stack=ph)
                lbv = sb("lbv", 128, stack=ph)
                oml = sb("oml", 128, stack=ph)

                for kc in range(8):
                    for hw_ in range(2):
                        i_ = kc * 2 + hw_
                        k_w = "winstg%d" % (i_ % 2)
                        DMA("sp" if i_ % 2 == 0 else "act", P.c(hw_ * 1034, hw_ * 1034 + 1034),
                            dap(w_in_c, l * D * NPROJ + kc * 128 * NPROJ + hw_ * 1034, [[NPROJ, 128], [1, 1034]]),
                            writes=[k_w])
                        CP("pool", w_in.ap(kc * WIN + hw_ * 1034, [[1, 1034]]), P.c(hw_ * 1034, hw_ * 1034 + 1034),
                           [k_w], ["w_in"])
                DMA("sp", gainb.ap(), dap(gain_c, l * 512, [[0, 128], [1, 512]]), writes=["gainb"])
                DMA("sp", convb.ap(), dap(conv_c, l * 1024, [[0, 128], [1, 1024]]), writes=["convb"])
                DMA("sp", gbb.ap(), dap(gateb_c, l * 4, [[0, 128], [1, 4]]), writes=["gbb"])
                DMA("sp", lb0.ap(), dap(hlb_c, 0, [[0, 128], [1, 128]]), writes=["lb0"])
                DMA("sp", lb1.ap(), dap(hlb_c, 128, [[0, 128], [1, 128]]), writes=["lb1"])
                DMA("sp", glaw.ap(0, [[1, 128]], 0, 17), dap(glaw2_c, l * 17 * 128, [[128, 17], [1, 128]]), writes=["glaw"])
                DMA("sp", gfull.c(0, 128), lgam_d.ap(), writes=["gfull_ret"])
                S.op("pool", lambda e: e.memset(lrT.ap(0, [[1, 128]], 0, 32), 1.0), writes=["lrT"])
                S.op("pool", lambda e: e.memset(state.ap(), 0.0), writes=["state"])
                S.op("pool", lambda e: e.memset(vx.ap(), 1.0), writes=["vx"])
                TT("dve", lbv.ap(), lb0.ap(), lb1.ap(), ALU.max, ["lb0", "lb1"], ["lbv"])
                TT("dve", lb0.ap(), lb0.ap(), lbv.ap(), ALU.subtract, ["lb0", "lbv"], ["lb0"])
                TT("dve", lb1.ap(), lb1.ap(), lbv.ap(), ALU.subtract, ["lb1", "lbv"], ["lb1"])
                ACT(lb0.ap(), lb0.ap(), AF.Exp, ["lb0"], ["lb0"])
                ACT(lb1.ap(), lb1.ap(), AF.Exp, ["lb1"], ["lb1"])
                TT("dve", lbv.ap(), lb0.ap(), lb1.ap(), ALU.add, ["lb0", "lb1"], ["lbv"])
                RECIP(lbv.ap(), lbv.ap(), ["lbv"], ["lbv"])
                if l == 0:
                    TS("dve", lbv.ap(), lbv.ap(), 0.0, None, ALU.mult, None, ["lbv"], ["lbv"])
                else:
                    TT("dve", lbv.ap(), lbv.ap(), lb1.ap(), ALU.mult, ["lbv", "lb1"], ["lbv"])
                TS("dve", oml.ap(), lbv.ap(), -1.0, 1.0, ALU.mult, ALU.add, ["lbv"], ["oml"])
                CP("dve", gg.c(128, 256), gainb.c(128, 256), ["gainb"], ["gg_c"])
                CP("dve", gg.c(384, 512), gainb.c(384, 512), ["gainb"], ["gg_c"])

                def tile_load(t_):
                    bb_ = t_ % 2
                    if li == 0:
                        DMA("sp", xt[bb_].ap(), dap(xfull, t_ * 128 * D, [[D, 128], [1, D]]), writes=["xt%d" % bb_])
                    else:
                        r_, tl_ = t_ // 16, t_ % 16
                        DMA("sp", xt[bb_].ap(), dap(xg_out[tl_ // 4], (r_ * 512 + (tl_ % 4) * 128) * D, [[D, 128], [1, D]]),
                            reads=["xg_out"], writes=["xt%d" % bb_])
                    DMA("sp", rot[bb_].ap(), dap(rot_d, t_ * 128 * 64, [[64, 128], [1, 64]]), writes=["rot%d" % bb_])

                def tile_front(t_, banks):
                    bb_ = t_ % 2
                    for kc in range(8):
                        bk = banks[kc // 4]
                        TR(pb[bk].c((kc % 4) * 128, (kc % 4) * 128 + 128), xt[bb_].c(kc * 128, kc * 128 + 128), ident,
                           ["xt%d" % bb_, "cst"], ["pb%d" % bk])
                    for kc in range(8):
                        bk = banks[kc // 4]
                        ACT(hT[bb_].c(kc * 128, kc * 128 + 128), pb[bk].c((kc % 4) * 128, (kc % 4) * 128 + 128), AF.Identity,
                            ["pb%d" % bk, "modT"], ["hT%d" % bb_],
                            scale=modT.c(kc, kc + 1), bias=modT.c(8 + kc, 8 + kc + 1))

                def tile_proj(t_):
                    bb_ = t_ % 2
                    for blk in range(5):
                        c0 = blk * 512
                        c1 = min(NPROJ, c0 + 512)
                        bank = 2 + blk if blk < 4 else 6
                        for kc in range(8):
                            MM(pb[bank].c(0, c1 - c0), hT[bb_].c(kc * 128, kc * 128 + 128),
                               w_in.ap(kc * WIN + c0, [[1, c1 - c0]]), ["hT%d" % bb_, "w_in"], ["pb%d" % bank],
                               start=(kc == 0), stop=(kc == 7))

                for ti in range(mix_tiles):
                    b2 = ti % 2
                    k_xt, k_rot, k_hT, k_xs = "xt%d" % b2, "rot%d" % b2, "hT%d" % b2, "xs%d" % b2
                    if ti == 0:
                        tile_load(0)
                        tile_front(0, (0, 1))
                        tile_proj(0)
                    if ti + 1 < mix_tiles:
                        tile_load(ti + 1)
                    ACT(P.c(384, 512), pb[2].c(384, 512), AF.Silu, ["pb2"], ["P_rg"])
                    ACT(P.c(1408, 1536), pb[4].c(384, 512), AF.Silu, ["pb4"], ["P_gg"])
                    ACT(qf.c(384, 512), pb[5].c(0, 128), AF.Silu, ["pb5"], ["qf_h"])
                    ACT(P.c(896, 1024), pb[3].c(384, 512), AF.Sigmoid, ["pb3"], ["P_mo"])
                    ACT(P.c(1920, 2048), pb[5].c(384, 512), AF.Sigmoid, ["pb5"], ["P_hg"])
                    CP("act", P.c(0, 256), pb[2].c(0, 256), ["pb2"], ["P_rqk"])
                    CP("act", vx.ap(0, [[65, 2], [1, 64]]), pb[2].ap(256, [[64, 2], [1, 64]]), ["pb2"], ["vx"])
                    CP("act", vx.ap(2 * 65, [[65, 2], [1, 64]]), pb[3].ap(256, [[64, 2], [1, 64]]), ["pb3"], ["vx"])
                    CP("act", vx.ap(4 * 65, [[65, 2], [1, 64]]), pb[4].ap(256, [[64, 2], [1, 64]]), ["pb4"], ["vx"])
                    CP("act", vx.ap(6 * 65, [[65, 2], [1, 64]]), pb[5].ap(256, [[64, 2], [1, 64]]), ["pb5"], ["vx"])
                    CP("act", qf.c(256, 384), pb[4].c(0, 128), ["pb4"], ["qf_g"])
                    ACT(kf.c(256, 384), pb[4].c(128, 256), AF.Copy, ["pb4"], ["kf_g"], scale=0.125)
                    CP("act", sm.ap(), pb[6].c(0, 20), ["pb6"], ["sm"])
                    CP("act", mqk.ap(), pb[3].c(0, 256), ["pb3"], ["mqk"])
                    for tap in range(4):
                        TT("pool", xs[b2].c(tap * 256, tap * 256 + 256), mqk.ap(),
                           convb.c(tap * 256, tap * 256 + 256), ALU.mult, ["mqk", "convb"], [k_xs])
                    import os as _os
                    _v = _os.environ.get("V459", "")
                    if _v == "copy":
                        ACT(htmp.ap(), pb[5].c(128, 256), AF.Copy, ["pb5"], ["htmp"], scale=-1.0)
                    elif _v == "noscale":
                        ACT(htmp.ap(), pb[5].c(128, 256), AF.Exp, ["pb5"], ["htmp"])
                    elif _v == "gtmp":
                        ACT(gtmp.ap(), pb[5].c(128, 256), AF.Exp, ["pb5"], ["gtmp"], scale=-1.0)
                    elif _v == "dve":
                        CP("dve", htmp.ap(), pb[5].c(128, 256), ["pb5"], ["htmp"])
                    elif _v == "poolms":
                        S.op("pool", lambda e: e.memset(htmp.ap(), 1.0), writes=["htmp"])
                    elif _v == "dvecst":
                        CP("dve", htmp.ap(), cst.c(0, 128), ["cst"], ["htmp"])
                    elif _v == "sbuf":
                        ACT(htmp.ap(), P.c(0, 128), AF.Exp, ["P_rqk"], ["htmp"], scale=-1.0)
                    else:
                        ACT(htmp.ap(), pb[5].c(128, 256), AF.Exp, ["pb5"], ["htmp"], scale=-1.0)
                    cosb = rot[b2].ap(0, [[0, 4], [1, 32]])
                    sinb = rot[b2].ap(32, [[0, 4], [1, 32]])
                    x1 = P.ap(0, [[64, 4], [1, 32]])
                    x2 = P.ap(32, [[64, 4], [1, 32]])
                    t_ = [rtmp.ap(i * 128, [[32, 4], [1, 32]]) for i in range(4)]
                    TT("pool", t_[0], x1, cosb, ALU.mult, ["P_rqk", k_rot], ["rtmp0"])
                    TT("pool", t_[1], x2, sinb, ALU.mult, ["P_rqk", k_rot], ["rtmp1"])
                    TT("pool", t_[2], x1, sinb, ALU.mult, ["P_rqk", k_rot], ["rtmp2"])
                    TT("pool", t_[3], x2, cosb, ALU.mult, ["P_rqk", k_rot], ["rtmp3"])
                    TT("pool", rq.ap(0, [[64, 4], [1, 32]]), t_[0], t_[1], ALU.subtract, ["rtmp0", "rtmp1"], ["rq"])
                    TT("pool", rq.ap(32, [[64, 4], [1, 32]]), t_[2], t_[3], ALU.add, ["rtmp2", "rtmp3"], ["rq"])
                    TS("pool", qf.c(0, 128), rq.c(0, 128), 0.125, None, ALU.mult, None, ["rq"], ["qf_r"])
                    CP("pool", kf.c(0, 128), rq.c(128, 256), ["rq"], ["kf_r"])
                    first = True
                    for sft in range(4):
                        tap = 3 - sft
                        lw = identb if sft == 0 else cstb.c(C_SH + (sft - 1) * 128, C_SH + sft * 128)
                        lastmm = (sft == 3) and (ti == 0)
                        MM(pb[7].c(0, 256), lw, xs[b2].c(tap * 256, tap * 256 + 256), [k_xs, "cstb"], ["pb7"],
                           start=first, stop=lastmm)
                        first = False
                        if sft > 0 and ti > 0:
                            MM(pb[7].c(0, 256), cstb.c(C_SP + (sft - 1) * 128, C_SP + sft * 128),
                               xs[1 - b2].c(tap * 256, tap * 256 + 256), ["xs%d" % (1 - b2), "cstb"], ["pb7"],
                               start=False, stop=(sft == 3))
                    TT("dve", gsm.c(0, 4), sm.c(0, 4), gbb.ap(), ALU.add, ["sm", "gbb"], ["gsm"])
                    TR(pb[7].ap(256, [[1, 128]], 0, 16), sm.c(4, 20), ident, ["sm", "cst"], ["pb7"])
                    CP("act", lrT.ap(0, [[1, 128]], 0, 16), pb[7].ap(256, [[1, 128]], 0, 16), ["pb7"], ["lrT"])
                    MM(pb[7].c(384, 512), lrT.ap(0, [[1, 128]], 0, 17), glaw.ap(0, [[1, 128]], 0, 17),
                       ["lrT", "glaw"], ["pb7"])
                    ACT(qkm.ap(), pb[7].c(0, 256), AF.Silu, ["pb7"], ["qkm"])
                    ACT(gsm.c(4, 6), gsm.c(0, 2), AF.Exp, ["gsm"], ["gsm_ei"])
                    ACT(gsm.c(6, 8), gsm.c(2, 4), AF.Exp, ["gsm"], ["gsm_ef"], scale=-1.0)
                    ACT(gtmp.ap(), pb[7].c(384, 512), AF.Exp, ["pb7"], ["gtmp"], scale=-1.0)
                    ACT(gsm.c(8, 10), gsm.c(6, 8), AF.Ln, ["gsm_ef"], ["gsm_lf"], bias=1.0)
                    ACT(gtmp.ap(), gtmp.ap(), AF.Ln, ["gtmp"], ["gtmp"], bias=1.0)
                    TS("dve", htmp.ap(), htmp.ap(), 1.0, None, ALU.add, None, ["htmp"], ["htmp"])
                    RECIP(htmp.ap(), htmp.ap(), ["htmp"], ["htmp"])
                    TT("dve", htmp.ap(), htmp.ap(), oml.ap(), ALU.mult, ["htmp", "oml"], ["htmp"])
                    TT("dve", hf.ap(), htmp.ap(), lbv.ap(), ALU.add, ["htmp", "lbv"], ["hf"])
                    ACT(gfull.c(384, 512), hf.ap(), AF.Ln, ["hf"], ["gfull_h"])
                    TS("dve", kf.c(384, 512), hf.ap(), -1.0, 1.0, ALU.mult, ALU.add, ["hf"], ["kf_h"])
                    TS("dve", gfull.ap(128, [[64, 2], [1, 64]]), gsm.ap(8, [[1, 2], [0, 64]]), -1.0, None, ALU.mult, None,
                       ["gsm_lf"], ["gfull_m"])
                    TS("dve", gfull.c(256, 384), gtmp.ap(), -1.0 / 16.0, None, ALU.mult, None, ["gtmp"], ["gfull_g"])
                    CP("pool", qf.c(128, 256), qkm.c(0, 128), ["qkm"], ["qf_m"])
                    STT(kf.ap(128, [[64, 2], [1, 64]]), qkm.ap(128, [[64, 2], [1, 64]]), 0.125,
                        gsm.ap(4, [[1, 2], [0, 64]]), ALU.mult, ALU.mult, ["qkm", "gsm_ei"], ["kf_m"])
                    gkeys = ["gfull_ret", "gfull_m", "gfull_g", "gfull_h"]
                    MM(pb[0].ap(), cst.c(C_LT, C_LT + 128), gfull.ap(), gkeys + ["cst"], ["pb0"])
                    for m in range(4):
                        MM(pb[1].c(m * 4, m * 4 + 4), gfull.c(m * 128, m * 128 + 128), cst.c(C_SEL, C_SEL + 4),
                           gkeys + ["cst"], ["pb1"])
                    ACT(eG.ap(), pb[0].ap(), AF.Exp, ["pb0"], ["eG"])
                    ACT(enG.ap(), pb[0].ap(), AF.Exp, ["pb0"], ["enG"], scale=-1.0)
                    ACT(acT.ap(), pb[1].c(0, 16), AF.Exp, ["pb1"], ["acT"])
                    qkeys = ["qf_r", "qf_m", "qf_g", "qf_h"]
                    kkeys = ["kf_r", "kf_m", "kf_g", "kf_h"]
                    TT("dve", Qt.ap(), qf.ap(), eG.ap(), ALU.mult, qkeys + ["eG"], ["Qt"])
                    TT("pool", Kt.ap(), kf.ap(), enG.ap(), ALU.mult, kkeys + ["enG"], ["Kt"])
                    for m in range(4):
                        TR(pbh[2].c(m * 128, m * 128 + 128), Qt.c(m * 128, m * 128 + 128), identb, ["Qt", "cstb"], ["pb2"])
                        TR(pbh[2].c(512 + m * 128, 512 + m * 128 + 128), Kt.c(m * 128, m * 128 + 128), identb,
                           ["Kt", "cstb"], ["pb2"])
                    if ti == 0:
                        dump("qfx%d" % l, qf.ap(), ["qf_r", "qf_m", "qf_g", "qf_h"])
                        dump("kfx%d" % l, kf.ap(), ["kf_r", "kf_m", "kf_g", "kf_h"])
                        dump("gfx%d" % l, gfull.ap(), ["gfull_ret", "gfull_m", "gfull_g", "gfull_h"])
                        dump("eG%d" % l, eG.ap(), ["eG"])
                        dump("enG%d" % l, enG.ap(), ["enG"])
                        dump("lbv%d" % l, lbv.ap(), ["lbv"])
                        dump("convb%d" % l, convb.ap(), ["convb"])
                        dump("gainb%d" % l, gainb.ap(), ["gainb"])
                    ACT(QT.ap(), pbh[2].c(0, 512), AF.Identity, ["pb2", "cst"], ["QT"],
                        scale=cst.c(C_HM, C_HM + 1), bias=cst.c(C_CM + 4, C_CM + 5))
                    ACT(QTb.ap(), pbh[2].c(0, 512), AF.Identity, ["pb2", "cst"], ["QTb"],
                        scale=cst.c(C_HM + 1, C_HM + 2), bias=cst.c(C_CM + 4, C_CM + 5))
                    CP("act", KT.ap(), pbh[2].c(512, 1024), ["pb2"], ["KT"])
                    for hh in range(8):
                        m, hf_ = hh // 2, hh % 2
                        bank = 3 + hh // 4
                        MM(pb[bank].c((hh % 4) * 128, (hh % 4) * 128 + 128),
                           KT.c(m * 128, m * 128 + 128), (QT if hf_ == 0 else QTb).c(m * 128, m * 128 + 128),
                           ["KT", "QT", "QTb"], ["pb%d" % bank])
                    maskb = cst.ap(C_MASK, [[0, 4], [1, 128]])
                    CP("act", Sraw.c(0, 512), pb[3].ap(), ["pb3"], ["Sraw0"])
                    CP("act", Sraw.c(512, 1024), pb[4].ap(), ["pb4"], ["Sraw1"])
                    TT("pool", Sm.ap(0, [[128, 4], [1, 128]]), Sraw.ap(0, [[128, 4], [1, 128]]), maskb, ALU.mult,
                       ["Sraw0", "cst"], ["Sm0"])
                    TT("pool", Sm.ap(512, [[128, 4], [1, 128]]), Sraw.ap(512, [[128, 4], [1, 128]]), maskb, ALU.mult,
                       ["Sraw1", "cst"], ["Sm1"])
                    DSB = (5, 6, 7, 2)
                    dsk = {5: ["pb5"], 6: ["pb6"], 7: ["pb7"], 2: ["pb2"]}
                    for c in range(4):
                        bank = DSB[c]
                        for hh in range(8):
                            m, hf_ = hh // 2, hh % 2
                            col = m * 65
                            MM(pb[bank].ap(col, [[1, 65]], hf_ * 64, 64),
                               Kt.ap(hh * 64, [[1, 64]], 32 * c, 32), vx.ap(hh * 65, [[1, 65]], 32 * c, 32),
                               ["Kt", "vx"], dsk[bank], tp=(32 * c, hf_ * 64))
                    for c in range(4):
                        bank = DSB[c]
                        CP("act", stb.c(c * 260, c * 260 + 260), state.ap(), ["state"], ["stb%d" % c])
                        TT("dve", stmp.ap(), state.ap(), pb[bank].c(0, 260), ALU.add,
                           ["state"] + dsk[bank], ["stmp"])
                        TT("dve", state.ap(0, [[65, 4], [1, 65]]), stmp.ap(0, [[65, 4], [1, 65]]),
                           acT.ap(c, [[4, 4], [0, 65]]), ALU.mult, ["stmp", "acT"], ["state"])
                    for hh in range(8):
                        m, hf_ = hh // 2, hh % 2
                        bank = hh // 4
                        col = (hh % 4) * 65
                        MM(pb[bank].c(col, col + 65), Sm.c(hh * 128, hh * 128 + 128), vx.c(hh * 65, hh * 65 + 65),
                           ["Sm0", "Sm1", "vx"], ["pb%d" % bank], start=True, stop=False)
                        for c in range(4):
                            MM(pb[bank].ap(col, [[1, 65]], 32 * c, 32),
                               (QT if hf_ == 0 else QTb).ap(m * 128 + 32 * c, [[1, 32]]),
                               stb.ap(c * 260 + m * 65, [[1, 65]]),
                               ["QT", "QTb", "stb%d" % c], ["pb%d" % bank], start=False, stop=True,
                               tp=(0, 32 * c))
                    CP("act", Ob.c(0, 260), pb[0].c(0, 260), ["pb0"], ["Ob0"])
                    CP("act", Ob.c(260, 520), pb[1].c(0, 260), ["pb1"], ["Ob1"])
                    if ti + 1 < mix_tiles:
                        tile_front(ti + 1, (3, 4))
                        tile_proj(ti + 1)
                    CP("pool", U.ap(0, [[64, 2], [1, 64]]), Ob.ap(0, [[65, 2], [1, 64]]), ["Ob0"], ["U_r"])
                    den_ = Ob.ap(2 * 65 + 64, [[65, 2]])
                    TS("dve", nsm.c(32, 34), den_, 1.0, None, ALU.max, None, ["Ob0"], ["nsm_dd"])
                    TS("dve", nsm.c(36, 38), den_, -1.0, 1.0, ALU.mult, ALU.max, ["Ob0"], ["nsm_dd2"])
                    TT("dve", nsm.c(32, 34), nsm.c(32, 34), nsm.c(36, 38), ALU.max, ["nsm_dd", "nsm_dd2"], ["nsm_dd"])
                    RECIP(nsm.c(34, 36), nsm.c(32, 34), ["nsm_dd"], ["nsm_rr"])
                    TT("dve", U.ap(128, [[64, 2], [1, 64]]), Ob.ap(2 * 65, [[65, 2], [1, 64]]),
                       nsm.ap(34, [[1, 2], [0, 64]]), ALU.mult, ["Ob0", "nsm_rr"], ["U_m"])
                    TT("pool", U.c(128, 256), U.c(128, 256), P.c(896, 1024), ALU.mult, ["U_m", "P_mo"], ["U_m"])
                    CP("pool", U.ap(256, [[64, 2], [1, 64]]), Ob.ap(260, [[65, 2], [1, 64]]), ["Ob1"], ["U_g"])
                    TT("dve", U.ap(384, [[64, 2], [1, 64]]), Ob.ap(260 + 2 * 65, [[65, 2], [1, 64]]),
                       P.ap(1920, [[64, 2], [1, 64]]), ALU.mult, ["Ob1", "P_hg"], ["U_h"])
                    ukeys = ["U_r", "U_m", "U_g", "U_h"]
                    RED(nsm.c(0, 8), U.ap(0, [[64, 8], [1, 64]]), ALU.add, ukeys, ["nsm_s1"])
                    TT("dve", nsm.c(8, 16), nsm.c(0, 8), cst.c(C_CM, C_CM + 8), ALU.mult, ["nsm_s1", "cst"], ["nsm_mean"])
                    TT("dve", Uc.ap(0, [[64, 8], [1, 64]]), U.ap(0, [[64, 8], [1, 64]]), nsm.ap(8, [[1, 8], [0, 64]]),
                       ALU.subtract, ukeys + ["nsm_mean"], ["Uc"])
                    TT("pool", sq.ap(), Uc.ap(), Uc.ap(), ALU.mult, ["Uc"], ["sq"])
                    RED(nsm.c(16, 24), sq.ap(0, [[64, 8], [1, 64]]), ALU.add, ["sq"], ["nsm_s2"])
                    TS("dve", nsm.c(24, 32), nsm.c(16, 24), 1.0 / 64.0, NORM_EPS, ALU.mult, ALU.add, ["nsm_s2"], ["nsm_r"])
                    ACT(nsm.c(24, 32), nsm.c(24, 32), AF.Sqrt, ["nsm_r"], ["nsm_r"])
                    RECIP(nsm.c(24, 32), nsm.c(24, 32), ["nsm_r"], ["nsm_r"])
                    TT("pool", gg.c(0, 128), P.c(384, 512), gainb.c(0, 128), ALU.mult, ["P_rg", "gainb"], ["gg_r"])
                    TT("pool", gg.c(256, 384), P.c(1408, 1536), gainb.c(256, 384), ALU.mult, ["P_gg", "gainb"], ["gg_g"])
                    TT("dve", Uc.ap(0, [[64, 8], [1, 64]]), Uc.ap(0, [[64, 8], [1, 64]]), nsm.ap(24, [[1, 8], [0, 64]]),
                       ALU.mult, ["Uc", "nsm_r"], ["Uc"])
                    TT("dve", Y[b2].ap(), Uc.ap(), gg.ap(), ALU.mult, ["Uc", "gg_r", "gg_g", "gg_c"], ["Y%d" % b2])
                    ydst = dap(yb_in[li][ti // 16], (ti % 16) * 128 * 512, [[512, 128], [1, 512]]) if fused else \
                        dap(yout_d, ti * 128 * 512, [[512, 128], [1, 512]])
                    DMA("sp", ydst, Y[b2].ap(), reads=["Y%d" % b2], writes=["yb_in_t%d" % ti])
                    if fused and ti == 15:
                        S.op("pool", lambda e, li=li: e.collective_compute(
                            "AllGather", ALU.bypass, replica_groups=pairs, ins=[yb_in[li][0].ap()], outs=[yb_out[li][0].ap()]),
                            reads=["yb_in_t%d" % t_ for t_ in range(16)], writes=["yb_out0"], cc=True)
                    if ti == 0:
                        dump("P%d" % l, P.ap(), ["P_rqk", "P_rg", "P_gg", "P_mo", "P_hg"])
                        dump("qf%d" % l, qf.ap(), qkeys)
                        dump("kf%d" % l, kf.ap(), kkeys)
                        dump("gfull%d" % l, gfull.ap(), gkeys)
                        dump("U%d" % l, U.ap(), ukeys)
                    if ti == 1:
                        dump("U1_%d" % l, U.ap(), ukeys)
                S.barrier()
                if fused:
                    S.op("pool", lambda e, li=li: e.collective_compute(
                        "AllGather", ALU.bypass, replica_groups=pairs, ins=[yb_in[li][1].ap()], outs=[yb_out[li][1].ap()]),
                        reads=[], writes=["yb_out1"], cc=True)
                S.emit()
            if stop_after == "mixer":
                break

            if not has_tok:
                break
            with ExitStack() as ph:
                h2T = sb("h2T", 8 * NOWN, BF16, stack=ph)
                wo = T(wbig.h, wbig.F, BF16)
                WO = 12288
                bst = sb("bst", 12, stack=ph)
                mv = sb("mv", 4, stack=ph)
                lng = sb("lng", D, stack=ph)
                lnb = sb("lnb", D, stack=ph)
                wst = [sb("wst%d" % i, D, stack=ph) for i in range(4)]
                ph1 = ExitStack()
                yin = [[sb("yin%d_%d" % (i, j), 512, BF16, stack=ph1) for j in range(4)] for i in range(2)]
                yT = sb("yT", 8 * 128, BF16, stack=ph1)
                z = sb("z", D, stack=ph1)
                h2f = sb("h2f", 8 * 128, stack=ph1)
                rt = sb("rt", 160, stack=ph1)

                DMA("sp", lng.ap(), dap(ln1_g, l * D, [[0, 128], [1, D]]), writes=["lng"])
                DMA("sp", lnb.ap(), dap(ln1_b, l * D, [[0, 128], [1, D]]), writes=["lnb"])
                wst_n = [0]
                for kc in range(8):
                    i_ = wst_n[0] % 4
                    wst_n[0] += 1
                    k_w = "wst%d" % i_
                    DMA("sp", wst[i_].ap(), dap(w_out_p, l * D * D + kc * 128 * D, [[D, 128], [1, D]]),
                        writes=[k_w])
                    TT("pool", wo.ap(WO + kc * D, [[1, D]]), wst[i_].ap(), gbc1.ap(), ALU.mult, [k_w, "gbc"], ["wo"])
                def load_expert_w13(e_, slot):
                    base = slot * 12288
                    for wi, wsrc in enumerate((exp_w1, exp_w3)):
                        for kp in range(4):
                            i_ = wst_n[0] % 4
                            wst_n[0] += 1
                            k_w = "wst%d" % i_
                            DMA("sp", wst[i_].ap(0, [[512, 2], [1, 512]]),
                                dap(wsrc, ((li * NE + e_) * D + kp * 256) * DE, [[DE, 128], [128 * DE, 2], [1, DE]]),
                                writes=[k_w])
                            CP("pool", wbig.ap(base + wi * 4096 + kp * 1024, [[1, 1024]]), wst[i_].ap(), [k_w],
                               ["w13_%d" % slot])
                load_expert_w13(0, 0)

                for tj in range(NTILE_TOK):
                    b2 = tj % 2
                    kx = "xres%d" % tj
                    xr = lambda c0, c1, tj=tj: xres.c(tj * D + c0, tj * D + c1)
                    for r in range(2):
                        for hf_ in range(2):
                            DMA("sp", yin[b2][r * 2 + hf_].ap(),
                                (dap(yb_out[li][hf_], (r * NOWN + tj * 128) * 512, [[512, 128], [1, 512]]) if fused else
                                 dap(ygath_d, ((r * SEQ) + hf_ * NOWN + tj * 128) * 512, [[512, 128], [1, 512]])),
                                reads=["yb_out%d" % hf_], writes=["yin%d" % b2])
                    for r in range(2):
                        for fb in range(4):
                            kc = r * 4 + fb
                            bank = kc // 4
                            for hf_ in range(2):
                                MM(pb[bank].c((kc % 4) * 128, (kc % 4) * 128 + 128),
                                   yin[b2][r * 2 + hf_].c(fb * 128, fb * 128 + 128),
                                   (ids0 if hf_ == 0 else ids1).ap(), ["yin%d" % b2, "ids0", "ids1"], ["pb%d" % bank],
                                   start=(hf_ == 0), stop=(hf_ == 1))
                    CP("act", yT.c(0, 512), pb[0].ap(), ["pb0"], ["yT"])
                    CP("act", yT.c(512, 1024), pb[1].ap(), ["pb1"], ["yT"])
                    for n in range(2):
                        for kc in range(8):
                            MM(pb[2 + n].ap(), yT.c(kc * 128, kc * 128 + 128), wo.ap(WO + kc * D + n * 512, [[1, 512]]),
                               ["yT", "wo"], ["pb%d" % (2 + n)], start=(kc == 0), stop=(kc == 7))
                    for n in range(2):
                        STT(z.c(n * 512, n * 512 + 512), xr(n * 512, n * 512 + 512), ALPHA, pb[2 + n].ap(), ALU.mult, ALU.add,
                            [kx, "pb%d" % (2 + n)], ["z"])
                    for n in range(2):
                        S.op("dve", lambda e, o=bst.c(n * 6, n * 6 + 6), i=z.c(n * 512, n * 512 + 512): e.bn_stats(o, i),
                             ["z"], ["bst"])
                    S.op("dve", lambda e, o=mv.c(0, 2), i=bst.ap(): e.bn_aggr(o, i), ["bst"], ["mv"])
                    TS("dve", mv.c(2, 3), mv.c(1, 2), LN_EPS, None, ALU.add, None, ["mv"], ["mv_r"])
                    ACT(mv.c(2, 3), mv.c(2, 3), AF.Sqrt, ["mv_r"], ["mv_r"])
                    RECIP(mv.c(2, 3), mv.c(2, 3), ["mv_r"], ["mv_r"])
                    TS("dve", z.ap(), z.ap(), mv.c(0, 1), mv.c(2, 3), ALU.subtract, ALU.mult, ["z", "mv", "mv_r"], ["z"])
                    TT("pool", z.ap(), z.ap(), lng.ap(), ALU.mult, ["z", "lng"], ["z"])
                    TT("dve", xr(0, D), z.ap(), lnb.ap(), ALU.add, ["z", "lnb"], [kx])
                    if tj == 0:
                        dump("x1_%d" % l, xr(0, D), [kx])
                    for kc in range(8):
                        bank = 4 + kc // 4
                        TR(pb[bank].c((kc % 4) * 128, (kc % 4) * 128 + 128), xr(kc * 128, kc * 128 + 128), ident,
                           [kx, "cst"], ["pb%d" % bank])
                    for kc in range(8):
                        bank = 4 + kc // 4
                        src = pb[bank].c((kc % 4) * 128, (kc % 4) * 128 + 128)
                        dst = h2f.c(kc * 128, kc * 128 + 128)
                        if kc % 2 == 0:
                            ACT(dst, src, AF.Identity, ["pb%d" % bank, "modT"], ["h2f"],
                                scale=modT.c(16 + kc, 17 + kc), bias=modT.c(24 + kc, 25 + kc))
                        else:
                            TS("dve", dst, src, modT.c(16 + kc, 17 + kc), modT.c(24 + kc, 25 + kc), ALU.mult, ALU.add,
                               ["pb%d" % bank, "modT"], ["h2f"])
                    CP("pool", h2T.ap(tj * 128, [[NOWN, 8], [1, 128]]), h2f.ap(0, [[128, 8], [1, 128]]), ["h2f"], ["h2T"])
                    for kc in range(8):
                        MM(pb[6].c(0, NE), h2f.c(kc * 128, kc * 128 + 128), rwf.c(kc * NE, kc * NE + NE), ["h2f", "rwf"],
                           ["pb6"], start=(kc == 0), stop=(kc == 7))
                    R = lambda a, b_: rt.c(a, b_)
                    R3 = lambda a: rt.ap(a, [[4, 4], [1, 4]])
                    BIG = 1.0e4
                    CP("act", R(128, 144), pb[6].c(0, NE), ["pb6"], ["rt_lg"])
                    RED(R(0, 1), R(128, 144), ALU.max, ["rt_lg"], ["rt_m"])
                    TS("dve", R(1, 2), R(0, 1), -1.0, None, ALU.mult, None, ["rt_m"], ["rt_nm"])
                    ACT(R(16, 32), R(128, 144), AF.Exp, ["rt_lg", "rt_nm"], ["rt_e"], bias=R(1, 2))
                    RED(R(2, 3), R(16, 32), ALU.add, ["rt_e"], ["rt_s"])
                    RECIP(R(2, 3), R(2, 3), ["rt_s"], ["rt_s"])
                    TS("dve", R(16, 32), R(16, 32), R(2, 3), None, ALU.mult, None, ["rt_e", "rt_s"], ["rt_p"])
                    TT("dve", R(32, 48), R(16, 32), rbb.ap(), ALU.add, ["rt_p", "rbb"], ["rt_sel"])
                    RED(R(4, 8), R3(32), ALU.max, ["rt_sel"], ["rt_g1"])
                    TT("dve", R3(48), R3(32), rt.ap(4, [[1, 4], [0, 4]]), ALU.is_equal, ["rt_sel", "rt_g1"], ["rt_eq"])
                    STT(R(48, 64), R(48, 64), -BIG, R(32, 48), ALU.mult, ALU.add, ["rt_eq", "rt_sel"], ["rt_v2"])
                    RED(R(8, 12), R3(48), ALU.max, ["rt_v2"], ["rt_g2"])
                    TT("dve", R(4, 8), R(4, 8), R(8, 12), ALU.add, ["rt_g1", "rt_g2"], ["rt_gs"])
                    RED(R(3, 4), R(4, 8), ALU.max, ["rt_gs"], ["rt_gm"])
                    TS("dve", R(8, 12), R(4, 8), R(3, 4), None, ALU.is_equal, None, ["rt_gs", "rt_gm"], ["rt_ing"])
                    TS("dve", R(8, 12), R(8, 12), -1.0, BIG, ALU.add, ALU.mult, ["rt_ing"], ["rt_pen"])
                    TT("dve", R3(64), R3(32), rt.ap(8, [[1, 4], [0, 4]]), ALU.add, ["rt_sel", "rt_pen"], ["rt_msk"])
                    RED(R(12, 13), R(64, 80), ALU.max, ["rt_msk"], ["rt_t1"])
                    TS("dve", R(80, 96), R(64, 80), R(12, 13), None, ALU.is_equal, None, ["rt_msk", "rt_t1"], ["rt_oh1"])
                    STT(R(96, 112), R(80, 96), -BIG, R(64, 80), ALU.mult, ALU.add, ["rt_oh1", "rt_msk"], ["rt_m2"])
                    RED(R(13, 14), R(96, 112), ALU.max, ["rt_m2"], ["rt_t2"])
                    TS("dve", R(112, 128), R(96, 112), R(13, 14), None, ALU.is_equal, None, ["rt_m2", "rt_t2"], ["rt_oh2"])
                    TT("dve", R(80, 96), R(80, 96), R(112, 128), ALU.add, ["rt_oh1", "rt_oh2"], ["rt_sm"])
                    TT("dve", R(80, 96), R(80, 96), R(16, 32), ALU.mult, ["rt_sm", "rt_p"], ["rt_pw"])
                    RED(R(14, 15), R(80, 96), ALU.add, ["rt_pw"], ["rt_ws"])
                    RECIP(R(14, 15), R(14, 15), ["rt_ws"], ["rt_ws"])
                    TS("dve", gates.c(tj * NE, tj * NE + NE), R(80, 96), R(14, 15), None, ALU.mult, None,
                       ["rt_pw", "rt_ws"], ["gates"])
                    if tj == 0:
                        dump("gates%d" % l, gates.c(0, NE), ["gates"])
                S.barrier()
                S.emit()
                ph1.close()
                if stop_after == "tok1":
                    break

                with ExitStack() as ph2:
                    sa = [sb("sa%d" % i, 512, stack=ph2) for i in range(2)]
                    sbb = [sb("sbb%d" % i, 512, stack=ph2) for i in range(2)]
                    yv = [sb("yv%d" % i, 512, stack=ph2) for i in range(2)]
                    gT = [sb("gT%d" % i, 4 * 512, BF16, stack=ph2) for i in range(2)]
                    DMA("sp", lng.ap(), dap(ln2_g, l * D, [[0, 128], [1, D]]), writes=["lng"])
                    DMA("sp", lnb.ap(), dap(ln2_b, l * D, [[0, 128], [1, D]]), writes=["lnb"])

                    def load_w2(e_, slot):
                        for fc in range(4):
                            i_ = wst_n[0] % 4
                            wst_n[0] += 1
                            k_w = "wst%d" % i_
                            DMA("sp", wst[i_].ap(),
                                dap(exp_w2, ((li * NE + e_) * DE + fc * 128) * D, [[D, 128], [1, D]]), writes=[k_w])
                            TT("pool", wbig.ap(slot * 12288 + 8192 + fc * D, [[1, D]]), wst[i_].ap(), gbc2.ap(), ALU.mult,
                               [k_w, "gbc"], ["w2b%d" % slot])
                    load_w2(0, 0)
                    cnt = [0]

                    def emit_w13(e_, tg):
                        slot = e_ % 2
                        base = slot * 12288
                        g_ = gT[tg % 2]
                        k_g = "gT%d" % (tg % 2)
                        for fb in range(4):
                            ba, bb = cnt[0] % 2, 2 + cnt[0] % 2
                            cnt[0] += 1
                            for kc in range(8):
                                MM(pb[ba].ap(), wbig.ap(base + kc * 512 + fb * 128, [[1, 128]]),
                                   h2T.ap(kc * NOWN + tg * 512, [[1, 512]]), ["w13_%d" % slot, "h2T"], ["pb%d" % ba],
                                   start=(kc == 0), stop=(kc == 7))
                            for kc in range(8):
                                MM(pb[bb].ap(), wbig.ap(base + 4096 + kc * 512 + fb * 128, [[1, 128]]),
                                   h2T.ap(kc * NOWN + tg * 512, [[1, 512]]), ["w13_%d" % slot, "h2T"], ["pb%d" % bb],
                                   start=(kc == 0), stop=(kc == 7))
                            ACT(sa[ba].ap(), pb[ba].ap(), AF.Silu, ["pb%d" % ba], ["sa%d" % ba])
                            CP("act", sbb[ba].ap(), pb[bb].ap(), ["pb%d" % bb], ["sbb%d" % ba])
                            TT("dve", g_.c(fb * 512, fb * 512 + 512), sa[ba].ap(), sbb[ba].ap(), ALU.mult,
                               ["sa%d" % ba, "sbb%d" % ba], [k_g])

                    def emit_w2(e_, tg):
                        slot = e_ % 2
                        base = slot * 12288
                        g_ = gT[tg % 2]
                        k_g = "gT%d" % (tg % 2)
                        for tt in range(4):
                            tile_ = tg * 4 + tt
                            kx = "xres%d" % tile_
                            for n in range(2):
                                by = 4 + (tt * 2 + n) % 4
                                for fc in range(4):
                                    MM(pb[by].ap(), g_.c(fc * 512 + tt * 128, fc * 512 + tt * 128 + 128),
                                       wbig.ap(base + 8192 + fc * D + n * 512, [[1, 512]]), [k_g, "w2b%d" % slot],
                                       ["pb%d" % by], start=(fc == 0), stop=(fc == 3))
                                xv = xres.c(tile_ * D + n * 512, tile_ * D + n * 512 + 512)
                                yi = (tt * 2 + n) % 2
                                ACT(yv[yi].ap(), pb[by].ap(), AF.Identity, ["pb%d" % by, "gates", "cst"], ["yv%d" % yi],
                                    scale=gates.c(tile_ * NE + e_, tile_ * NE + e_ + 1), bias=cst.c(C_CM + 4, C_CM + 5))
                                TT("dve", xv, xv, yv[yi].ap(), ALU.add, ["yv%d" % yi, kx], [kx])

                    steps = [(e_, tg) for e_ in range(NE) for tg in range(4)]
                    for k, (e_, tg) in enumerate(steps):
                        emit_w13(e_, tg)
                        if k > 0:
                            emit_w2(*steps[k - 1])
                        if tg == 0 and e_ + 1 < NE:
                            load_expert_w13(e_ + 1, 1 - e_ % 2)
                            load_w2(e_ + 1, 1 - e_ % 2)
                    emit_w2(*steps[-1])
                    for tj in range(NTILE_TOK):
                        kx = "xres%d" % tj
                        xr = lambda c0, c1, tj=tj: xres.c(tj * D + c0, tj * D + c1)
                        if tj == 0:
                            dump("z2_%d" % l, xr(0, D), [kx])
                        for n in range(2):
                            S.op("dve", lambda e, o=bst.c(n * 6, n * 6 + 6), i=xr(n * 512, n * 512 + 512): e.bn_stats(o, i),
                                 [kx], ["bst"])
                        S.op("dve", lambda e, o=mv.c(0, 2), i=bst.ap(): e.bn_aggr(o, i), ["bst"], ["mv"])
                        TS("dve", mv.c(2, 3), mv.c(1, 2), LN_EPS / (ALPHA * ALPHA), None, ALU.add, None, ["mv"], ["mv_r"])
                        ACT(mv.c(2, 3), mv.c(2, 3), AF.Sqrt, ["mv_r"], ["mv_r"])
                        RECIP(mv.c(2, 3), mv.c(2, 3), ["mv_r"], ["mv_r"])
                        TS("dve", xr(0, D), xr(0, D), mv.c(0, 1), mv.c(2, 3), ALU.subtract, ALU.mult, [kx, "mv", "mv_r"], [kx])
                        TT("pool", xr(0, D), xr(0, D), lng.ap(), ALU.mult, [kx, "lng"], [kx])
                        TT("dve", xr(0, D), xr(0, D), lnb.ap(), ALU.add, [kx, "lnb"], [kx])
                        xdst = dap(out_d, tj * 128 * D, [[D, 128], [1, D]]) if last else \
                            dap(xg_in[tj // 4], (tj % 4) * 128 * D, [[D, 128], [1, D]])
                        DMA("sp", xdst, xr(0, D), reads=[kx], writes=["xg_in"])
                    S.barrier()
                    if not last:
                        for q in range(4):
                            S.op("pool", lambda e, q=q: e.collective_compute(
                                "AllGather", ALU.bypass, replica_groups=pairs, ins=[xg_in[q].ap()], outs=[xg_out[q].ap()]),
                                reads=["xg_in"], writes=["xg_out"], cc=True)
                    S.emit()
        print("bass program: %d scheduled ops" % S.n_inst)
    return nc


W = 256
_SPLITS = (W, W, W, W, W, W, W, W, 4, 4, W, W, W, W, 16, W, W, W, W)
_OFFS = np.concatenate([[0], np.cumsum(_SPLITS)]).astype(int)
_WIDE = [0, 1, 2, 3, 4, 5, 6, 7, 10, 11, 12, 13, 15, 16, 17, 18]


def _consts():
    c = np.zeros((128, NCONST), np.float32)
    i = np.arange(128)
    c[i, C_ID + i] = 1.0
    jj, ii = np.meshgrid(i, i, indexing="ij")
    same = (jj // 32) == (ii // 32)
    lt = (same & (jj <= ii)).astype(np.float32)
    c[:, C_LT:C_LT + 128] = lt
    c[:, C_MASK:C_MASK + 128] = lt
    for cc in range(4):
        c[cc * 32:(cc + 1) * 32, C_SEL + cc] = 1.0
    for s in (1, 2, 3):
        c[:, C_SH + (s - 1) * 128:C_SH + s * 128] = (jj == ii - s)
        c[:, C_SP + (s - 1) * 128:C_SP + s * 128] = (jj == 128 + ii - s)
    c[:, C_CM:C_CM + 4] = 1.0 / 64.0
    c[:64, C_HM] = 1.0
    c[64:, C_HM + 1] = 1.0
    return c


def _rot_table():
    inv = (10000.0 ** (-np.arange(0, 64, 2, dtype=np.float32) / np.float32(64))).astype(np.float32)
    ang = np.arange(SEQ, dtype=np.float32)[:, None] * inv[None, :]
    return np.concatenate([np.cos(ang), np.sin(ang)], axis=1).astype(np.float32)


def make_in_maps(inp, layer, part, x_cur, ys=None, n_cores=8, layers=None):
    f = lambda a: np.ascontiguousarray(np.asarray(a, dtype=np.float32))
    x, c = f(x_cur), f(inp["c"])
    w_in = f(inp["w_in"])
    consts = _consts()
    rot = _rot_table()
    ls = [layer] if layers is None else list(layers)
    shared = {
        "ada_w": np.ascontiguousarray(f(inp["ada_w"])[ls]),
        "ada_bT": np.ascontiguousarray(f(inp["ada_b"]).reshape(DEPTH, 48, 128).transpose(0, 2, 1)),
        "ada_bg": np.ascontiguousarray(np.stack([f(inp["ada_b"])[:, 2048:3072], f(inp["ada_b"])[:, 5120:6144]], axis=1)),
        "ln1_g": f(inp["ln1_g"]), "ln1_b": f(inp["ln1_b"]), "ln2_g": f(inp["ln2_g"]), "ln2_b": f(inp["ln2_b"]),
        "router_w": f(inp["router_w"]), "router_b": f(inp["router_b"]).reshape(1, NE),
        "consts": consts, "rot": rot,
    }
    if part in ("tok", "fused"):
        shared["exp_w1"] = np.ascontiguousarray(f(inp["exp_w1"])[ls])
        shared["exp_w3"] = np.ascontiguousarray(f(inp["exp_w3"])[ls])
        shared["exp_w2"] = np.ascontiguousarray(f(inp["exp_w2"])[ls])
    per_s = []
    for s in range(2):
        hs = slice(2 * s * 64, (2 * s + 2) * 64)
        cols = []
        for q in _WIDE:
            cols.append(np.arange(_OFFS[q] + 2 * s * 64, _OFFS[q] + (2 * s + 2) * 64))
        cols.append(np.arange(_OFFS[8] + 2 * s, _OFFS[8] + 2 * s + 2))
        cols.append(np.arange(_OFFS[9] + 2 * s, _OFFS[9] + 2 * s + 2))
        cols.append(np.arange(_OFFS[14], _OFFS[14] + 16))
        cols = np.concatenate(cols)
        conv = f(inp["mlstm_conv"])
        gb = f(inp["mlstm_gate_b"])
        perm = np.concatenate([np.arange(m * 256 + r * 128, m * 256 + r * 128 + 128) for r in range(2) for m in range(4)])
        lg = np.zeros((128, 128), np.float32)
        for hl in range(2):
            lg[:, hl * 64:(hl + 1) * 64] = np.float32(np.log1p(-(2.0 ** (-5.0 - (2 * s + hl)))))
        per_s.append({
            "w_in_c": np.ascontiguousarray(w_in[:, :, cols]),
            "conv_c": np.ascontiguousarray(np.concatenate([conv[:, :, hs], conv[:, :, 256:][:, :, hs]], axis=2)),
            "gateb_c": np.ascontiguousarray(np.concatenate([gb[:, 2 * s:2 * s + 2], gb[:, 4 + 2 * s:4 + 2 * s + 2]], axis=1)),
            "glaw2_c": np.ascontiguousarray(np.concatenate([f(inp["gla_w2"])[:, :, hs], f(inp["gla_b2"])[:, None, hs]], axis=1)),
            "hlb_c": np.ascontiguousarray(f(inp["hgrn_lb"])[:, hs]),
            "gain_c": np.ascontiguousarray(np.concatenate([f(inp["ret_norm"])[:, hs], f(inp["mlstm_norm"])[:, hs],
                                                           f(inp["gla_norm"])[:, hs], f(inp["hgrn_norm"])[:, hs]], axis=1)),
            "w_out_p": np.ascontiguousarray(f(inp["w_out"])[:, perm, :]),
            "lgam": lg,
            "sel": np.ascontiguousarray(np.tile(np.array([[1.0 - s, float(s)]], np.float32), (128, 1))),
        })
    maps = []
    for core in range(n_cores):
        b, s = core // 2, core % 2
        m = dict(shared)
        m.update(per_s[s])
        if part in ("mix", "fused"):
            m["xfull"] = np.ascontiguousarray(x[b])
        if part == "tok":
            m["ygath"] = np.ascontiguousarray(np.concatenate([np.asarray(ys[2 * b]), np.asarray(ys[2 * b + 1])], axis=0))
        m["xown"] = np.ascontiguousarray(x[b, s * NOWN:(s + 1) * NOWN])
        m["cT"] = np.ascontiguousarray(c[b].reshape(8, 128).T)
        maps.append(m)
    return maps


def run_layer(inputs, l, x_cur, n_cores=8, dbg=None):
    cores = list(range(n_cores))
    nc = build(layer=l, part="mix", n_cores=n_cores)
    res = run_bass_kernel_spmd(nc, make_in_maps(inputs, l, "mix", x_cur, n_cores=n_cores), core_ids=cores)
    ys = [res.results[c]["yout"] for c in cores]
    nc2 = build(layer=l, part="tok", n_cores=n_cores, dbg=dbg)
    res2 = run_bass_kernel_spmd(nc2, make_in_maps(inputs, l, "tok", x_cur, ys, n_cores=n_cores), core_ids=cores)
    x_new = np.array(x_cur, dtype=np.float32, copy=True)
    for core in cores:
        b, s = core // 2, core % 2
        x_new[b, s * NOWN:(s + 1) * NOWN] = res2.results[core]["out"]
    return x_new, res2


def run_fused(inputs, layers, x_cur, n_cores=8, dbg=None):
    cores = list(range(n_cores))
    nc = build(part="fused", layers=layers, n_cores=n_cores, dbg=dbg)
    res = run_bass_kernel_spmd(nc, make_in_maps(inputs, layers[0], "fused", x_cur, n_cores=n_cores, layers=layers),
                               core_ids=cores)
    x_new = np.array(x_cur, dtype=np.float32, copy=True)
    for core in cores:
        b, s = core // 2, core % 2
        x_new[b, s * NOWN:(s + 1) * NOWN] = res.results[core]["out"]
    return x_new, res


def kernel(**inputs):
    x_cur = np.asarray(inputs["x"], dtype=np.float32)
    x_cur, _ = run_fused(inputs, list(range(DEPTH)), x_cur)
    return x_cur
```

```python
import math
from contextlib import ExitStack

import numpy as np
import ml_dtypes

import concourse.bass as bass
import concourse.mybir as mybir
from concourse.alu_op_type import AluOpType as ALU
from concourse.bass_utils import run_bass_kernel_spmd

F32 = mybir.dt.float32
BF16 = mybir.dt.bfloat16
AF = mybir.ActivationFunctionType
AX = mybir.AxisListType

D = 1024
SEQ = 4096
NB = 4
DEPTH = 2
NE = 16
DE = 512
ALPHA = (2 * DEPTH) ** 0.25
LN_EPS = 1e-5
NORM_EPS = 1e-6
NTILE_MIX = SEQ // 128
NOWN = SEQ // 2
NTILE_TOK = NOWN // 128
NPROJ = 2068
ENGS = ("pe", "dve", "act", "pool", "sp")

C_ID, C_LT, C_MASK, C_SEL = 0, 128, 256, 384
C_SH = 388
C_SP = C_SH + 3 * 128
C_CM = C_SP + 3 * 128
C_HM = C_CM + 8
NCONST = C_HM + 2


class Sched:
    def __init__(self, nc, es, n_dma_sems=16):
        self.nc = nc
        self.ops = {e: [] for e in ENGS}
        self.sem = {e: es.enter_context(nc.semaphore("s_" + e)) for e in ENGS}
        self.cnt = {e: 0 for e in ENGS}
        self.dma_sems = [[es.enter_context(nc.semaphore("d_%d" % i)), 0, None] for i in range(n_dma_sems)]
        self.cc_sems = []
        self.es = es
        self.dma_rr = 0
        self.res = {}
        self.waited = {e: {} for e in ENGS}
        self.n_inst = 0

    def _need(self, eng, tok, waits):
        if tok is None:
            return
        sem, val, src = tok
        if src == eng and eng == "pe":
            return
        key = id(sem)
        if self.waited[eng].get(key, 0) >= val:
            return
        if key in waits:
            val = max(val, waits[key][1])
        waits[key] = (sem, val)

    def op(self, eng, fn, reads=(), writes=(), dma=False, cc=False):
        import os as _os
        if self.n_inst >= int(_os.environ.get("MAXOPS", "100000000")):
            if not getattr(self, "_delayed", False) and _os.environ.get("DELAYN"):
                self._delayed = True
                for _i in range(int(_os.environ["DELAYN"])):
                    self.cnt["act"] += 1
                    self.ops["act"].append(([], self.delay_fn, (self.sem["act"], 1)))
            return None
        if str(self.n_inst) in _os.environ.get("SKIPOPS", "").split(","):
            self.n_inst += 1
            return None
        if _os.environ.get("OPTRACE"):
            import inspect
            fr = inspect.stack()
            ln = [f.lineno for f in fr[1:4]]
            print("OP", self.n_inst, eng, ln, "dma" if dma else "")
        waits = {}
        for k in reads:
            st = self.res.get(k)
            if st is not None:
                self._need(eng, st["w"], waits)
        for k in writes:
            st = self.res.get(k)
            if st is not None:
                self._need(eng, st["w"], waits)
                for t in st["r"]:
                    if t[2] != eng or dma or cc:
                        self._need(eng, t, waits)
        if cc:
            sem = self.es.enter_context(self.nc.semaphore("cc_%d" % len(self.cc_sems)))
            self.cc_sems.append(sem)
            tok = (sem, 1, "cc")
            inc = (sem, None)
        elif dma:
            slot = self.dma_sems[self.dma_rr]
            self.dma_rr = (self.dma_rr + 1) % len(self.dma_sems)
            self._need(eng, slot[2], waits)
            slot[1] += 16
            tok = (slot[0], slot[1], "dma")
            slot[2] = tok
            inc = (slot[0], 16)
        else:
            self.cnt[eng] += 1
            tok = (self.sem[eng], self.cnt[eng], eng)
            inc = (self.sem[eng], 1)
        wl = list(waits.values())
        for sem, val in wl:
            self.waited[eng][id(sem)] = val
        self.ops[eng].append((wl, fn, inc))
        for k in reads:
            st = self.res.setdefault(k, {"w": None, "r": []})
            st["r"].append(tok)
        for k in writes:
            self.res[k] = {"w": tok, "r": []}
        self.n_inst += 1
        return tok

    def barrier(self):
        toks = [(self.sem[e], self.cnt[e], e) for e in ENGS if self.cnt[e] > 0]
        toks += [(s[0], s[1], "dma") for s in self.dma_sems if s[1] > 0]
        toks += [(s, 1, "cc") for s in self.cc_sems]
        for e in ENGS:
            waits = {}
            for t in toks:
                if t[2] == e:
                    continue
                key = id(t[0])
                if self.waited[e].get(key, 0) >= t[1]:
                    continue
                waits[key] = (t[0], t[1])
                self.waited[e][key] = t[1]
            if waits:
                self.ops[e].append((list(waits.values()), None, None))
        self.res = {}

    def emit(self):
        nc = self.nc
        ops = self.ops
        self.ops = {e: [] for e in ENGS}
        with nc.Block() as block:
            def mk(ename):
                def body(e):
                    for waits, fn, inc in ops[ename]:
                        for sem, val in waits:
                            e.wait_ge(sem, val)
                        if fn is not None:
                            ins = fn(e)
                            if inc[1] is None:
                                ins.then_inc(inc[0])
                            else:
                                ins.then_inc(inc[0], inc[1])
                return body
            block.tensor(mk("pe"))
            block.vector(mk("dve"))
            block.scalar(mk("act"))
            block.gpsimd(mk("pool"))
            block.sync(mk("sp"))


class T:
    def __init__(self, h, F, dt):
        self.h, self.F, self.dt = h, F, dt

    def ap(self, off=0, dims=None, p0=0, n=128):
        if dims is None:
            dims = [[1, self.F - off]]
        return bass.AP(self.h, p0 * self.F + off, [[self.F, n]] + [list(d) for d in dims])

    def c(self, c0, c1, p0=0, n=128):
        return self.ap(c0, [[1, c1 - c0]], p0, n)


def dap(h, off, dims):
    return bass.AP(h, off, [list(d) for d in dims])


def build(layer=0, part="mix", dbg=None, n_cores=8, stop_after=None, mix_tiles=NTILE_MIX, layers=None):
    nc = bass.Bass("TRN2", target_bir_lowering=False)
    dbg = dbg or {}

    def din(name, shape, dt=F32):
        return nc.dram_tensor(name, list(shape), dt, kind="ExternalInput")

    layers = [layer] if layers is None else list(layers)
    nl = len(layers)
    fused = part == "fused"
    has_mix = part in ("mix", "fused")
    has_tok = part in ("tok", "fused")
    pairs = [[2 * i, 2 * i + 1] for i in range(n_cores // 2)]
    xfull = din("xfull", [SEQ, D]) if has_mix else None
    xown = din("xown", [NOWN, D])
    cT_d = din("cT", [128, 8])
    ada_w = din("ada_w", [nl, D, 6 * D])
    ada_bT = din("ada_bT", [DEPTH, 128, 48])
    ada_bg = din("ada_bg", [DEPTH, 2, D])
    w_in_c = din("w_in_c", [DEPTH, D, NPROJ])
    conv_c = din("conv_c", [DEPTH, 4, 256])
    gateb_c = din("gateb_c", [DEPTH, 4])
    glaw2_c = din("glaw2_c", [DEPTH, 17, 128])
    hlb_c = din("hlb_c", [DEPTH, 128])
    gain_c = din("gain_c", [DEPTH, 512])
    w_out_p = din("w_out_p", [DEPTH, D, D])
    ln1_g = din("ln1_g", [DEPTH, D])
    ln1_b = din("ln1_b", [DEPTH, D])
    ln2_g = din("ln2_g", [DEPTH, D])
    ln2_b = din("ln2_b", [DEPTH, D])
    router_w = din("router_w", [D, NE])
    router_b = din("router_b", [1, NE])
    if has_tok:
        exp_w1 = din("exp_w1", [nl, NE, D, DE])
        exp_w3 = din("exp_w3", [nl, NE, D, DE])
        exp_w2 = din("exp_w2", [nl, NE, DE, D])
    consts_d = din("consts", [128, NCONST])
    lgam_d = din("lgam", [128, 128])
    rot_d = din("rot", [SEQ, 64])
    sel_d = din("sel", [128, 2])
    if part == "mix":
        yout_d = nc.dram_tensor("yout", [SEQ, 512], BF16, kind="ExternalOutput")
    if part == "tok":
        ygath_d = din("ygath", [2 * SEQ, 512], BF16)
    if has_tok:
        out_d = nc.dram_tensor("out", [NOWN, D], F32, kind="ExternalOutput")
    if fused:
        yb_in = [[nc.dram_tensor("yb_in%d_%d" % (i, h), [NOWN, 512], BF16) for h in range(2)] for i in range(nl)]
        yb_out = [[nc.dram_tensor("yb_out%d_%d" % (i, h), [2 * NOWN, 512], BF16) for h in range(2)] for i in range(nl)]
        xg_in = [nc.dram_tensor("xg_in%d" % q, [512, D], F32) for q in range(4)]
        xg_out = [nc.dram_tensor("xg_out%d" % q, [1024, D], F32) for q in range(4)]
    dbg_out = {k: nc.dram_tensor("dbg_" + k, list(v), F32, kind="ExternalOutput") for k, v in dbg.items()}


    with ExitStack() as es:
        S = Sched(nc, es)

        uniq = [0]

        def sb(name, F, dt=F32, stack=es):
            uniq[0] += 1
            return T(stack.enter_context(nc.sbuf_tensor("sb%d_%s" % (uniq[0], name), [128, F], dt)), F, dt)

        def DMA(eng, out, in_, reads=(), writes=()):
            S.op(eng, lambda e, o=out, i=in_: e.dma_start(out=o, in_=i), reads, writes, dma=True)

        def MM(out, lhsT, rhs, reads, writes, start=True, stop=True, tp=None):
            S.op("pe", lambda e, o=out, l=lhsT, r=rhs, a=start, b=stop, t=tp:
                 e.matmul(o, l, r, start=a, stop=b, tile_position=t), reads, writes)

        def TR(out, in_, ident, reads, writes):
            S.op("pe", lambda e, o=out, i=in_, d=ident: e.transpose(o, i, d), reads, writes)

        def ACT(out, in_, func, reads, writes, scale=None, bias=None):
            kw = {}
            if scale is not None:
                kw["scale"] = scale
            if bias is not None:
                kw["bias"] = bias
            S.op("act", lambda e, o=out, i=in_, f=func, k=kw: e.activation(o, i, f, **k), reads, writes)

        def TT(eng, out, in0, in1, op, reads, writes):
            S.op(eng, lambda e, o=out, a=in0, b=in1, p=op: e.tensor_tensor(o, a, b, p), reads, writes)

        def TS(eng, out, in0, s1, s2, op0, op1, reads, writes):
            if op1 is None:
                S.op(eng, lambda e, o=out, a=in0, x=s1, p=op0: e.tensor_scalar(o, a, x, None, p), reads, writes)
            else:
                S.op(eng, lambda e, o=out, a=in0, x=s1, y=s2, p=op0, q=op1:
                     e.tensor_scalar(o, a, x, y, p, q), reads, writes)

        def STT(out, in0, scalar, in1, op0, op1, reads, writes):
            S.op("dve", lambda e, o=out, a=in0, s=scalar, b=in1, p=op0, q=op1:
                 e.scalar_tensor_tensor(o, a, s, b, p, q), reads, writes)

        def CP(eng, out, in_, reads, writes):
            if eng == "act":
                ACT(out, in_, AF.Copy, reads, writes)
            else:
                S.op(eng, lambda e, o=out, i=in_: e.tensor_copy(o, i), reads, writes)

        def RED(out, in_, op, reads, writes):
            S.op("dve", lambda e, o=out, i=in_, p=op: e.tensor_reduce(o, i, AX.X, p), reads, writes)

        def RECIP(out, in_, reads, writes):
            S.op("dve", lambda e, o=out, i=in_: e.reciprocal(o, i), reads, writes)

        def dump(name, src_ap, reads):
            if name in dbg_out:
                shp = dbg[name]
                dims = [[int(np.prod(shp[1:])), shp[0]]]
                if len(shp) == 2:
                    dims.append([1, shp[1]])
                else:
                    dims += [[int(np.prod(shp[i + 1:])), shp[i]] for i in range(1, len(shp))]
                DMA("sp", dap(dbg_out[name], 0, dims), src_ap, reads=reads)

        xres = sb("xres", NTILE_TOK * D)
        S.delay_fn = lambda e: e.activation(xres.c(15 * D, 16 * D), xres.c(15 * D, 16 * D), AF.Copy)
        cst = sb("cst", NCONST)
        cstb = sb("cstb", NCONST, BF16)
        sel = sb("sel", 2)
        ids0 = sb("ids0", 128, BF16)
        ids1 = sb("ids1", 128, BF16)
        ones1 = sb("ones1", 128)
        condT = sb("condT", 8)
        modT = sb("modT", 32)
        gbc1 = sb("gbc1", D)
        gbc2 = sb("gbc2", D)
        rwf = sb("rwf", 8 * NE)
        rbb = sb("rbb", NE)
        gates = sb("gates", NTILE_TOK * NE)
        wbig = sb("wbig", 2 * 12288, BF16)
        pb = [T(es.enter_context(nc.psum_tensor("pb%d" % i, [128, 512], F32)), 512, F32) for i in range(8)]
        pbh = [T(p.h.bitcast(BF16), 1024, BF16) for p in pb]

        ident = cst.c(C_ID, C_ID + 128)
        identb = cstb.c(C_ID, C_ID + 128)

        DMA("sp", cst.ap(), consts_d.ap(), writes=["cst"])
        DMA("sp", sel.ap(), sel_d.ap(), writes=["sel"])
        DMA("sp", condT.ap(), cT_d.ap(), writes=["condT"])
        DMA("sp", rwf.ap(0, [[NE, 8], [1, NE]]), dap(router_w, 0, [[NE, 128], [128 * NE, 8], [1, NE]]), writes=["rwf"])
        DMA("sp", rbb.ap(), dap(router_b, 0, [[0, 128], [1, NE]]), writes=["rbb"])
        for t in range(NTILE_TOK):
            DMA("sp" if t % 2 == 0 else "act", xres.c(t * D, (t + 1) * D),
                dap(xown, t * 128 * D, [[D, 128], [1, D]]), writes=["xres%d" % t])
        CP("dve", cstb.ap(), cst.ap(), ["cst"], ["cstb"])
        TS("dve", ids0.ap(), cst.c(C_ID, C_ID + 128), sel.c(0, 1), None, ALU.mult, None, ["cst", "sel"], ["ids0"])
        TS("dve", ids1.ap(), cst.c(C_ID, C_ID + 128), sel.c(1, 2), None, ALU.mult, None, ["cst", "sel"], ["ids1"])
        S.op("pool", lambda e: e.memset(ones1.ap(), 1.0), writes=["ones1"])
        ACT(condT.ap(), condT.ap(), AF.Silu, ["condT"], ["condT"])
        S.barrier()
        S.emit()

        for li, l in enumerate(layers):
            last = li == nl - 1
            if stop_after == "setup":
                break
            with ExitStack() as ph:
                stg = [sb("adastg%d" % i, 8 * 512, stack=ph) for i in range(2)]
                abT = sb("abT", 48, stack=ph)
                abg = sb("abg", 2 * D, stack=ph)
                grow = sb("grow", 2 * D, stack=ph)
                DMA("sp", abT.ap(), dap(ada_bT, l * 128 * 48, [[48, 128], [1, 48]]), writes=["abT"])
                DMA("sp", abg.ap(0, [[1, 2 * D]], 0, 1), dap(ada_bg, l * 2 * D, [[0, 1], [1, 2 * D]]), writes=["abg"])
                kind_of = {0: 1, 1: 0, 3: 3, 4: 2}
                for hc in range(12):
                    ch, half = hc // 2, hc % 2
                    st_ = stg[hc % 2]
                    k_st = "adastg%d" % (hc % 2)
                    DMA("sp" if hc % 2 == 0 else "act", st_.ap(0, [[512, 8], [1, 512]]),
                        dap(ada_w, li * D * 6 * D + hc * 512, [[6 * D, 128], [128 * 6 * D, 8], [1, 512]]),
                        writes=[k_st])
                    if ch in kind_of:
                        kind = kind_of[ch]
                        for j in range(4):
                            jj = half * 4 + j
                            for kc in range(8):
                                MM(pb[0].ap(jj, [[1, 1]]), st_.ap(kc * 512 + j * 128, [[1, 128]]),
                                   condT.c(kc, kc + 1), [k_st, "condT"], ["pb0"], start=(kc == 0), stop=(kc == 7))
                        if half == 1:
                            TT("dve", modT.c(kind * 8, kind * 8 + 8), pb[0].c(0, 8), abT.c(ch * 8, ch * 8 + 8),
                               ALU.add, ["pb0", "abT"], ["modT"])
                            if kind in (0, 2):
                                TS("dve", modT.c(kind * 8, kind * 8 + 8), modT.c(kind * 8, kind * 8 + 8), 1.0, None,
                                   ALU.add, None, ["modT"], ["modT"])
                    else:
                        gi = 0 if ch == 2 else 1
                        for kc in range(8):
                            MM(pb[1].ap(0, [[1, 512]], 0, 1), condT.c(kc, kc + 1), st_.ap(kc * 512, [[1, 512]]),
                               [k_st, "condT"], ["pb1"], start=(kc == 0), stop=(kc == 7))
                        TT("dve", grow.ap(gi * D + half * 512, [[1, 512]], 0, 1), pb[1].ap(0, [[1, 512]], 0, 1),
                           abg.ap(gi * D + half * 512, [[1, 512]], 0, 1), ALU.add, ["pb1", "abg"], ["grow"])
                TS("dve", grow.ap(D, [[1, D]], 0, 1), grow.ap(D, [[1, D]], 0, 1), 1.0 / ALPHA, None, ALU.mult, None,
                   ["grow"], ["grow"])
                for gi, gb_ in enumerate((gbc1, gbc2)):
                    for half in range(2):
                        MM(pb[2 + half].ap(), ones1.ap(0, [[1, 128]], 0, 1),
                           grow.ap(gi * D + half * 512, [[1, 512]], 0, 1), ["ones1", "grow"], ["pb%d" % (2 + half)])
                        CP("dve", gb_.c(half * 512, half * 512 + 512), pb[2 + half].ap(), ["pb%d" % (2 + half)],
                           ["gbc"])
                dump("modT%d" % l, modT.ap(), ["modT"])
                dump("gbc1_%d" % l, gbc1.ap(), ["gbc"])
                S.barrier()
                S.emit()
            if stop_after == "mod":
                break

            if not has_mix:
                pass
            else:
              with ExitStack() as ph:
                w_in = T(wbig.h, wbig.F, BF16)
                WIN = NPROJ
                xt = [sb("xt%d" % i, D, stack=ph) for i in range(2)]
                rot = [sb("rot%d" % i, 64, stack=ph) for i in range(2)]
                hT = [sb("hT%d" % i, 8 * 128, BF16, stack=ph) for i in range(2)]
                P = sb("P", NPROJ, stack=ph)
                rq = sb("rq", 256, stack=ph)
                rtmp = sb("rtmp", 4 * 128, stack=ph)
                qf = sb("qf", 512, stack=ph)
                kf = sb("kf", 512, stack=ph)
                vx = sb("vx", 8 * 65 + 8, BF16, stack=ph)
                gfull = sb("gfull", 512, stack=ph)
                eG = sb("eG", 512, stack=ph)
                enG = sb("enG", 512, stack=ph)
                Qt = sb("Qt", 512, BF16, stack=ph)
                Kt = sb("Kt", 512, BF16, stack=ph)
                QT = sb("QT", 512, BF16, stack=ph)
                QTb = sb("QTb", 512, BF16, stack=ph)
                KT = sb("KT", 512, BF16, stack=ph)
                Sm = sb("Sm", 8 * 128, BF16, stack=ph)
                Sraw = sb("Sraw", 8 * 128, BF16, stack=ph)
                acT = sb("acT", 16, stack=ph)
                state = sb("state", 4 * 65, stack=ph)
                stmp = sb("stmp", 4 * 65, stack=ph)
                stb = sb("stb", 4 * 4 * 65 + 8, BF16, stack=ph)
                xs = [sb("xs%d" % i, 4 * 256, BF16, stack=ph) for i in range(2)]
                qkm = sb("qkm", 256, stack=ph)
                mqk = sb("mqk", 256, stack=ph)
                sm = sb("sm", 20, stack=ph)
                gsm = sb("gsm", 16, stack=ph)
                lrT = sb("lrT", 128, stack=ph)
                glaw = sb("glaw", 128, stack=ph)
                gtmp = sb("gtmp", 128, stack=ph)
                htmp = sb("htmp", 128, stack=ph)
                hf = sb("hf", 128, stack=ph)
                U = sb("U", 512, stack=ph)
                Ob = sb("Ob", 520, stack=ph)
                Uc = sb("Uc", 512, stack=ph)
                sq = sb("sq", 512, stack=ph)
                gg = sb("gg", 512, stack=ph)
                nsm = sb("nsm", 40, stack=ph)
                Y = [sb("Y%d" % i, 512, BF16, stack=ph) for i in range(2)]
                gainb = sb("gainb", 512, stack=ph)
                convb = sb("convb", 4 * 256, stack=ph)
                gbb = sb("gbb", 4, stack=ph)
                lb0 = sb("lb0", 128, stack=ph)
                lb1 = sb("lb1", 128, stack=ph)
                lbv = sb("lbv", 128, stack=ph)
                oml = sb("oml", 128, stack=ph)

                for kc in range(8):
                    for hw_ in range(2):
                        i_ = kc * 2 + hw_
                        k_w = "winstg%d" % (i_ % 2)
                        DMA("sp" if i_ % 2 == 0 else "act", P.c(hw_ * 1034, hw_ * 1034 + 1034),
                            dap(w_in_c, l * D * NPROJ + kc * 128 * NPROJ + hw_ * 1034, [[NPROJ, 128], [1, 1034]]),
                            writes=[k_w])
                        CP("pool", w_in.ap(kc * WIN + hw_ * 1034, [[1, 1034]]), P.c(hw_ * 1034, hw_ * 1034 + 1034),
                           [k_w], ["w_in"])
                DMA("sp", gainb.ap(), dap(gain_c, l * 512, [[0, 128], [1, 512]]), writes=["gainb"])
                DMA("sp", convb.ap(), dap(conv_c, l * 1024, [[0, 128], [1, 1024]]), writes=["convb"])
                DMA("sp", gbb.ap(), dap(gateb_c, l * 4, [[0, 128], [1, 4]]), writes=["gbb"])
                DMA("sp", lb0.ap(), dap(hlb_c, 0, [[0, 128], [1, 128]]), writes=["lb0"])
                DMA("sp", lb1.ap(), dap(hlb_c, 128, [[0, 128], [1, 128]]), writes=["lb1"])
                DMA("sp", glaw.ap(0, [[1, 128]], 0, 17), dap(glaw2_c, l * 17 * 128, [[128, 17], [1, 128]]), writes=["glaw"])
                DMA("sp", gfull.c(0, 128), lgam_d.ap(), writes=["gfull_ret"])
                S.op("pool", lambda e: e.memset(lrT.ap(0, [[1, 128]], 0, 32), 1.0), writes=["lrT"])
                S.op("pool", lambda e: e.memset(state.ap(), 0.0), writes=["state"])
                S.op("pool", lambda e: e.memset(vx.ap(), 1.0), writes=["vx"])
                TT("dve", lbv.ap(), lb0.ap(), lb1.ap(), ALU.max, ["lb0", "lb1"], ["lbv"])
                TT("dve", lb0.ap(), lb0.ap(), lbv.ap(), ALU.subtract, ["lb0", "lbv"], ["lb0"])
                TT("dve", lb1.ap(), lb1.ap(), lbv.ap(), ALU.subtract, ["lb1", "lbv"], ["lb1"])
                ACT(lb0.ap(), lb0.ap(), AF.Exp, ["lb0"], ["lb0"])
                ACT(lb1.ap(), lb1.ap(), AF.Exp, ["lb1"], ["lb1"])
                TT("dve", lbv.ap(), lb0.ap(), lb1.ap(), ALU.add, ["lb0", "lb1"], ["lbv"])
                RECIP(lbv.ap(), lbv.ap(), ["lbv"], ["lbv"])
                if l == 0:
                    TS("dve", lbv.ap(), lbv.ap(), 0.0, None, ALU.mult, None, ["lbv"], ["lbv"])
                else:
                    TT("dve", lbv.ap(), lbv.ap(), lb1.ap(), ALU.mult, ["lbv", "lb1"], ["lbv"])
                TS("dve", oml.ap(), lbv.ap(), -1.0, 1.0, ALU.mult, ALU.add, ["lbv"], ["oml"])
                CP("dve", gg.c(128, 256), gainb.c(128, 256), ["gainb"], ["gg_c"])
                CP("dve", gg.c(384, 512), gainb.c(384, 512), ["gainb"], ["gg_c"])

                def tile_load(t_):
                    bb_ = t_ % 2
                    if li == 0:
                        DMA("sp", xt[bb_].ap(), dap(xfull, t_ * 128 * D, [[D, 128], [1, D]]), writes=["xt%d" % bb_])
                    else:
                        r_, tl_ = t_ // 16, t_ % 16
                        DMA("sp", xt[bb_].ap(), dap(xg_out[tl_ // 4], (r_ * 512 + (tl_ % 4) * 128) * D, [[D, 128], [1, D]]),
                            reads=["xg_out"], writes=["xt%d" % bb_])
                    DMA("sp", rot[bb_].ap(), dap(rot_d, t_ * 128 * 64, [[64, 128], [1, 64]]), writes=["rot%d" % bb_])

                def tile_front(t_, banks):
                    bb_ = t_ % 2
                    for kc in range(8):
                        bk = banks[kc // 4]
                        TR(pb[bk].c((kc % 4) * 128, (kc % 4) * 128 + 128), xt[bb_].c(kc * 128, kc * 128 + 128), ident,
                           ["xt%d" % bb_, "cst"], ["pb%d" % bk])
                    for kc in range(8):
                        bk = banks[kc // 4]
                        ACT(hT[bb_].c(kc * 128, kc * 128 + 128), pb[bk].c((kc % 4) * 128, (kc % 4) * 128 + 128), AF.Identity,
                            ["pb%d" % bk, "modT"], ["hT%d" % bb_],
                            scale=modT.c(kc, kc + 1), bias=modT.c(8 + kc, 8 + kc + 1))

                def tile_proj(t_):
                    bb_ = t_ % 2
                    for blk in range(5):
                        c0 = blk * 512
                        c1 = min(NPROJ, c0 + 512)
                        bank = 2 + blk if blk < 4 else 6
                        for kc in range(8):
                            MM(pb[bank].c(0, c1 - c0), hT[bb_].c(kc * 128, kc * 128 + 128),
                               w_in.ap(kc * WIN + c0, [[1, c1 - c0]]), ["hT%d" % bb_, "w_in"], ["pb%d" % bank],
                               start=(kc == 0), stop=(kc == 7))

                for ti in range(mix_tiles):
                    b2 = ti % 2
                    k_xt, k_rot, k_hT, k_xs = "xt%d" % b2, "rot%d" % b2, "hT%d" % b2, "xs%d" % b2
                    if ti == 0:
                        tile_load(0)
                        tile_front(0, (0, 1))
                        tile_proj(0)
                    if ti + 1 < mix_tiles:
                        tile_load(ti + 1)
                    CP("act", mqk.ap(), pb[3].c(0, 256), ["pb3"], ["mqk"])
                    CP("act", sm.ap(), pb[6].c(0, 20), ["pb6"], ["sm"])
                    CP("act", P.c(0, 256), pb[2].c(0, 256), ["pb2"], ["P_rqk"])
                    ACT(P.c(384, 512), pb[2].c(384, 512), AF.Silu, ["pb2"], ["P_rg"])
                    ACT(P.c(1408, 1536), pb[4].c(384, 512), AF.Silu, ["pb4"], ["P_gg"])
                    ACT(qf.c(384, 512), pb[5].c(0, 128), AF.Silu, ["pb5"], ["qf_h"])
                    ACT(P.c(896, 1024), pb[3].c(384, 512), AF.Sigmoid, ["pb3"], ["P_mo"])
                    ACT(P.c(1920, 2048), pb[5].c(384, 512), AF.Sigmoid, ["pb5"], ["P_hg"])
                    CP("act", vx.ap(0, [[65, 2], [1, 64]]), pb[2].ap(256, [[64, 2], [1, 64]]), ["pb2"], ["vx"])
                    CP("act", vx.ap(2 * 65, [[65, 2], [1, 64]]), pb[3].ap(256, [[64, 2], [1, 64]]), ["pb3"], ["vx"])
                    CP("act", vx.ap(4 * 65, [[65, 2], [1, 64]]), pb[4].ap(256, [[64, 2], [1, 64]]), ["pb4"], ["vx"])
                    CP("act", vx.ap(6 * 65, [[65, 2], [1, 64]]), pb[5].ap(256, [[64, 2], [1, 64]]), ["pb5"], ["vx"])
                    CP("act", qf.c(256, 384), pb[4].c(0, 128), ["pb4"], ["qf_g"])
                    ACT(kf.c(256, 384), pb[4].c(128, 256), AF.Copy, ["pb4"], ["kf_g"], scale=0.125)
                    for tap in range(4):
                        TT("pool", xs[b2].c(tap * 256, tap * 256 + 256), mqk.ap(),
                           convb.c(tap * 256, tap * 256 + 256), ALU.mult, ["mqk", "convb"], [k_xs])
                    import os as _os
                    _v = _os.environ.get("V459", "")
                    if _v == "copy":
                        ACT(htmp.ap(), pb[5].c(128, 256), AF.Copy, ["pb5"], ["htmp"], scale=-1.0)
                    elif _v == "noscale":
                        ACT(htmp.ap(), pb[5].c(128, 256), AF.Exp, ["pb5"], ["htmp"])
                    elif _v == "gtmp":
                        ACT(gtmp.ap(), pb[5].c(128, 256), AF.Exp, ["pb5"], ["gtmp"], scale=-1.0)
                    elif _v == "dve":
                        CP("dve", htmp.ap(), pb[5].c(128, 256), ["pb5"], ["htmp"])
                    elif _v == "poolms":
                        S.op("pool", lambda e: e.memset(htmp.ap(), 1.0), writes=["htmp"])
                    elif _v == "dvecst":
                        CP("dve", htmp.ap(), cst.c(0, 128), ["cst"], ["htmp"])
                    elif _v == "sbuf":
                        ACT(htmp.ap(), P.c(0, 128), AF.Exp, ["P_rqk"], ["htmp"], scale=-1.0)
                    else:
                        ACT(htmp.ap(), pb[5].c(128, 256), AF.Exp, ["pb5"], ["htmp"], scale=-1.0)
                    cosb = rot[b2].ap(0, [[0, 4], [1, 32]])
                    sinb = rot[b2].ap(32, [[0, 4], [1, 32]])
                    x1 = P.ap(0, [[64, 4], [1, 32]])
                    x2 = P.ap(32, [[64, 4], [1, 32]])
                    t_ = [rtmp.ap(i * 128, [[32, 4], [1, 32]]) for i in range(4)]
                    TT("pool", t_[0], x1, cosb, ALU.mult, ["P_rqk", k_rot], ["rtmp0"])
                    TT("pool", t_[1], x2, sinb, ALU.mult, ["P_rqk", k_rot], ["rtmp1"])
                    TT("pool", t_[2], x1, sinb, ALU.mult, ["P_rqk", k_rot], ["rtmp2"])
                    TT("pool", t_[3], x2, cosb, ALU.mult, ["P_rqk", k_rot], ["rtmp3"])
                    TT("pool", rq.ap(0, [[64, 4], [1, 32]]), t_[0], t_[1], ALU.subtract, ["rtmp0", "rtmp1"], ["rq"])
                    TT("pool", rq.ap(32, [[64, 4], [1, 32]]), t_[2], t_[3], ALU.add, ["rtmp2", "rtmp3"], ["rq"])
                    TS("pool", qf.c(0, 128), rq.c(0, 128), 0.125, None, ALU.mult, None, ["rq"], ["qf_r"])
                    CP("pool", kf.c(0, 128), rq.c(128, 256), ["rq"], ["kf_r"])
                    first = True
                    for sft in range(4):
                        tap = 3 - sft
                        lw = identb if sft == 0 else cstb.c(C_SH + (sft - 1) * 128, C_SH + sft * 128)
                        lastmm = (sft == 3) and (ti == 0)
                        MM(pb[7].c(0, 256), lw, xs[b2].c(tap * 256, tap * 256 + 256), [k_xs, "cstb"], ["pb7"],
                           start=first, stop=lastmm)
                        first = False
                        if sft > 0 and ti > 0:
                            MM(pb[7].c(0, 256), cstb.c(C_SP + (sft - 1) * 128, C_SP + sft * 128),
                               xs[1 - b2].c(tap * 256, tap * 256 + 256), ["xs%d" % (1 - b2), "cstb"], ["pb7"],
                               start=False, stop=(sft == 3))
                    TT("dve", gsm.c(0, 4), sm.c(0, 4), gbb.ap(), ALU.add, ["sm", "gbb"], ["gsm"])
                    TR(pb[7].ap(256, [[1, 128]], 0, 16), sm.c(4, 20), ident, ["sm", "cst"], ["pb7"])
                    CP("act", lrT.ap(0, [[1, 128]], 0, 16), pb[7].ap(256, [[1, 128]], 0, 16), ["pb7"], ["lrT"])
                    MM(pb[7].c(384, 512), lrT.ap(0, [[1, 128]], 0, 17), glaw.ap(0, [[1, 128]], 0, 17),
                       ["lrT", "glaw"], ["pb7"])
                    ACT(qkm.ap(), pb[7].c(0, 256), AF.Silu, ["pb7"], ["qkm"])
                    ACT(gsm.c(4, 6), gsm.c(0, 2), AF.Exp, ["gsm"], ["gsm_ei"])
                    ACT(gsm.c(6, 8), gsm.c(2, 4), AF.Exp, ["gsm"], ["gsm_ef"], scale=-1.0)
                    ACT(gtmp.ap(), pb[7].c(384, 512), AF.Exp, ["pb7"], ["gtmp"], scale=-1.0)
                    ACT(gsm.c(8, 10), gsm.c(6, 8), AF.Ln, ["gsm_ef"], ["gsm_lf"], bias=1.0)
                    ACT(gtmp.ap(), gtmp.ap(), AF.Ln, ["gtmp"], ["gtmp"], bias=1.0)
                    TS("dve", htmp.ap(), htmp.ap(), 1.0, None, ALU.add, None, ["htmp"], ["htmp"])
                    RECIP(htmp.ap(), htmp.ap(), ["htmp"], ["htmp"])
                    TT("dve", htmp.ap(), htmp.ap(), oml.ap(), ALU.mult, ["htmp", "oml"], ["htmp"])
                    TT("dve", hf.ap(), htmp.ap(), lbv.ap(), ALU.add, ["htmp", "lbv"], ["hf"])
                    ACT(gfull.c(384, 512), hf.ap(), AF.Ln, ["hf"], ["gfull_h"])
                    TS("dve", kf.c(384, 512), hf.ap(), -1.0, 1.0, ALU.mult, ALU.add, ["hf"], ["kf_h"])
                    TS("dve", gfull.ap(128, [[64, 2], [1, 64]]), gsm.ap(8, [[1, 2], [0, 64]]), -1.0, None, ALU.mult, None,
                       ["gsm_lf"], ["gfull_m"])
                    TS("dve", gfull.c(256, 384), gtmp.ap(), -1.0 / 16.0, None, ALU.mult, None, ["gtmp"], ["gfull_g"])
                    CP("pool", qf.c(128, 256), qkm.c(0, 128), ["qkm"], ["qf_m"])
                    STT(kf.ap(128, [[64, 2], [1, 64]]), qkm.ap(128, [[64, 2], [1, 64]]), 0.125,
                        gsm.ap(4, [[1, 2], [0, 64]]), ALU.mult, ALU.mult, ["qkm", "gsm_ei"], ["kf_m"])
                    gkeys = ["gfull_ret", "gfull_m", "gfull_g", "gfull_h"]
                    MM(pb[0].ap(), cst.c(C_LT, C_LT + 128), gfull.ap(), gkeys + ["cst"], ["pb0"])
                    for m in range(4):
                        MM(pb[1].c(m * 4, m * 4 + 4), gfull.c(m * 128, m * 128 + 128), cst.c(C_SEL, C_SEL + 4),
                           gkeys + ["cst"], ["pb1"])
                    ACT(eG.ap(), pb[0].ap(), AF.Exp, ["pb0"], ["eG"])
                    ACT(enG.ap(), pb[0].ap(), AF.Exp, ["pb0"], ["enG"], scale=-1.0)
                    ACT(acT.ap(), pb[1].c(0, 16), AF.Exp, ["pb1"], ["acT"])
                    qkeys = ["qf_r", "qf_m", "qf_g", "qf_h"]
                    kkeys = ["kf_r", "kf_m", "kf_g", "kf_h"]
                    TT("dve", Qt.ap(), qf.ap(), eG.ap(), ALU.mult, qkeys + ["eG"], ["Qt"])
                    TT("pool", Kt.ap(), kf.ap(), enG.ap(), ALU.mult, kkeys + ["enG"], ["Kt"])
                    for m in range(4):
                        TR(pbh[2].c(m * 128, m * 128 + 128), Qt.c(m * 128, m * 128 + 128), identb, ["Qt", "cstb"], ["pb2"])
                        TR(pbh[2].c(512 + m * 128, 512 + m * 128 + 128), Kt.c(m * 128, m * 128 + 128), identb,
                           ["Kt", "cstb"], ["pb2"])
                    if ti == 0:
                        dump("qfx%d" % l, qf.ap(), ["qf_r", "qf_m", "qf_g", "qf_h"])
                        dump("kfx%d" % l, kf.ap(), ["kf_r", "kf_m", "kf_g", "kf_h"])
                        dump("gfx%d" % l, gfull.ap(), ["gfull_ret", "gfull_m", "gfull_g", "gfull_h"])
                        dump("eG%d" % l, eG.ap(), ["eG"])
                        dump("enG%d" % l, enG.ap(), ["enG"])
                        dump("lbv%d" % l, lbv.ap(), ["lbv"])
                        dump("convb%d" % l, convb.ap(), ["convb"])
                        dump("gainb%d" % l, gainb.ap(), ["gainb"])
                    ACT(QT.ap(), pbh[2].c(0, 512), AF.Identity, ["pb2", "cst"], ["QT"],
                        scale=cst.c(C_HM, C_HM + 1), bias=cst.c(C_CM + 4, C_CM + 5))
                    ACT(QTb.ap(), pbh[2].c(0, 512), AF.Identity, ["pb2", "cst"], ["QTb"],
                        scale=cst.c(C_HM + 1, C_HM + 2), bias=cst.c(C_CM + 4, C_CM + 5))
                    CP("act", KT.ap(), pbh[2].c(512, 1024), ["pb2"], ["KT"])
                    for hh in range(8):
                        m, hf_ = hh // 2, hh % 2
                        bank = 3 + hh // 4
                        MM(pb[bank].c((hh % 4) * 128, (hh % 4) * 128 + 128),
                           KT.c(m * 128, m * 128 + 128), (QT if hf_ == 0 else QTb).c(m * 128, m * 128 + 128),
                           ["KT", "QT", "QTb"], ["pb%d" % bank])
                    maskb = cst.ap(C_MASK, [[0, 4], [1, 128]])
                    CP("act", Sraw.c(0, 512), pb[3].ap(), ["pb3"], ["Sraw0"])
                    CP("act", Sraw.c(512, 1024), pb[4].ap(), ["pb4"], ["Sraw1"])
                    TT("pool", Sm.ap(0, [[128, 4], [1, 128]]), Sraw.ap(0, [[128, 4], [1, 128]]), maskb, ALU.mult,
                       ["Sraw0", "cst"], ["Sm0"])
                    TT("pool", Sm.ap(512, [[128, 4], [1, 128]]), Sraw.ap(512, [[128, 4], [1, 128]]), maskb, ALU.mult,
                       ["Sraw1", "cst"], ["Sm1"])
                    DSB = (5, 6, 7, 2)
                    dsk = {5: ["pb5"], 6: ["pb6"], 7: ["pb7"], 2: ["pb2"]}
                    for c in range(4):
                        bank = DSB[c]
                        for hh in range(8):
                            m, hf_ = hh // 2, hh % 2
                            col = m * 65
                            MM(pb[bank].ap(col, [[1, 65]], hf_ * 64, 64),
                               Kt.ap(hh * 64, [[1, 64]], 32 * c, 32), vx.ap(hh * 65, [[1, 65]], 32 * c, 32),
                               ["Kt", "vx"], dsk[bank], tp=(32 * c, hf_ * 64))
                    for c in range(4):
                        bank = DSB[c]
                        CP("act", stb.c(c * 260, c * 260 + 260), state.ap(), ["state"], ["stb%d" % c])
                        TT("dve", stmp.ap(), state.ap(), pb[bank].c(0, 260), ALU.add,
                           ["state"] + dsk[bank], ["stmp"])
                        TT("dve", state.ap(0, [[65, 4], [1, 65]]), stmp.ap(0, [[65, 4], [1, 65]]),
                           acT.ap(c, [[4, 4], [0, 65]]), ALU.mult, ["stmp", "acT"], ["state"])
                    for hh in range(8):
                        m, hf_ = hh // 2, hh % 2
                        bank = hh // 4
                        col = (hh % 4) * 65
                        MM(pb[bank].c(col, col + 65), Sm.c(hh * 128, hh * 128 + 128), vx.c(hh * 65, hh * 65 + 65),
                           ["Sm0", "Sm1", "vx"], ["pb%d" % bank], start=True, stop=False)
                        for c in range(4):
                            MM(pb[bank].ap(col, [[1, 65]], 32 * c, 32),
                               (QT if hf_ == 0 else QTb).ap(m * 128 + 32 * c, [[1, 32]]),
                               stb.ap(c * 260 + m * 65, [[1, 65]]),
                               ["QT", "QTb", "stb%d" % c], ["pb%d" % bank], start=False, stop=True,
                               tp=(0, 32 * c))
                    CP("act", Ob.c(0, 260), pb[0].c(0, 260), ["pb0"], ["Ob0"])
                    CP("act", Ob.c(260, 520), pb[1].c(0, 260), ["pb1"], ["Ob1"])
                    if ti + 1 < mix_tiles:
                        tile_front(ti + 1, (3, 4))
                        tile_proj(ti + 1)
                    CP("pool", U.ap(0, [[64, 2], [1, 64]]), Ob.ap(0, [[65, 2], [1, 64]]), ["Ob0"], ["U_r"])
                    den_ = Ob.ap(2 * 65 + 64, [[65, 2]])
                    TS("dve", nsm.c(32, 34), den_, 1.0, None, ALU.max, None, ["Ob0"], ["nsm_dd"])
                    TS("dve", nsm.c(36, 38), den_, -1.0, 1.0, ALU.mult, ALU.max, ["Ob0"], ["nsm_dd2"])
                    TT("dve", nsm.c(32, 34), nsm.c(32, 34), nsm.c(36, 38), ALU.max, ["nsm_dd", "nsm_dd2"], ["nsm_dd"])
                    RECIP(nsm.c(34, 36), nsm.c(32, 34), ["nsm_dd"], ["nsm_rr"])
                    TT("dve", U.ap(128, [[64, 2], [1, 64]]), Ob.ap(2 * 65, [[65, 2], [1, 64]]),
                       nsm.ap(34, [[1, 2], [0, 64]]), ALU.mult, ["Ob0", "nsm_rr"], ["U_m"])
                    TT("pool", U.c(128, 256), U.c(128, 256), P.c(896, 1024), ALU.mult, ["U_m", "P_mo"], ["U_m"])
                    CP("pool", U.ap(256, [[64, 2], [1, 64]]), Ob.ap(260, [[65, 2], [1, 64]]), ["Ob1"], ["U_g"])
                    TT("dve", U.ap(384, [[64, 2], [1, 64]]), Ob.ap(260 + 2 * 65, [[65, 2], [1, 64]]),
                       P.ap(1920, [[64, 2], [1, 64]]), ALU.mult, ["Ob1", "P_hg"], ["U_h"])
                    ukeys = ["U_r", "U_m", "U_g", "U_h"]
                    RED(nsm.c(0, 8), U.ap(0, [[64, 8], [1, 64]]), ALU.add, ukeys, ["nsm_s1"])
                    TT("dve", nsm.c(8, 16), nsm.c(0, 8), cst.c(C_CM, C_CM + 8), ALU.mult, ["nsm_s1", "cst"], ["nsm_mean"])
                    TT("dve", Uc.ap(0, [[64, 8], [1, 64]]), U.ap(0, [[64, 8], [1, 64]]), nsm.ap(8, [[1, 8], [0, 64]]),
                       ALU.subtract, ukeys + ["nsm_mean"], ["Uc"])
                    TT("pool", sq.ap(), Uc.ap(), Uc.ap(), ALU.mult, ["Uc"], ["sq"])
                    RED(nsm.c(16, 24), sq.ap(0, [[64, 8], [1, 64]]), ALU.add, ["sq"], ["nsm_s2"])
                    TS("dve", nsm.c(24, 32), nsm.c(16, 24), 1.0 / 64.0, NORM_EPS, ALU.mult, ALU.add, ["nsm_s2"], ["nsm_r"])
                    ACT(nsm.c(24, 32), nsm.c(24, 32), AF.Sqrt, ["nsm_r"], ["nsm_r"])
                    RECIP(nsm.c(24, 32), nsm.c(24, 32), ["nsm_r"], ["nsm_r"])
                    TT("pool", gg.c(0, 128), P.c(384, 512), gainb.c(0, 128), ALU.mult, ["P_rg", "gainb"], ["gg_r"])
                    TT("pool", gg.c(256, 384), P.c(1408, 1536), gainb.c(256, 384), ALU.mult, ["P_gg", "gainb"], ["gg_g"])
                    TT("dve", Uc.ap(0, [[64, 8], [1, 64]]), Uc.ap(0, [[64, 8], [1, 64]]), nsm.ap(24, [[1, 8], [0, 64]]),
                       ALU.mult, ["Uc", "nsm_r"], ["Uc"])
                    TT("dve", Y[b2].ap(), Uc.ap(), gg.ap(), ALU.mult, ["Uc", "gg_r", "gg_g", "gg_c"], ["Y%d" % b2])
                    ydst = dap(yb_in[li][ti // 16], (ti % 16) * 128 * 512, [[512, 128], [1, 512]]) if fused else \
                        dap(yout_d, ti * 128 * 512, [[512, 128], [1, 512]])
                    DMA("sp", ydst, Y[b2].ap(), reads=["Y%d" % b2], writes=["yb_in_t%d" % ti])
                    if fused and ti == 15:
                        S.op("pool", lambda e, li=li: e.collective_compute(
                            "AllGather", ALU.bypass, replica_groups=pairs, ins=[yb_in[li][0].ap()], outs=[yb_out[li][0].ap()]),
                            reads=["yb_in_t%d" % t_ for t_ in range(16)], writes=["yb_out0"], cc=True)
                    if ti == 0:
                        dump("P%d" % l, P.ap(), ["P_rqk", "P_rg", "P_gg", "P_mo", "P_hg"])
                        dump("qf%d" % l, qf.ap(), qkeys)
                        dump("kf%d" % l, kf.ap(), kkeys)
                        dump("gfull%d" % l, gfull.ap(), gkeys)
                        dump("U%d" % l, U.ap(), ukeys)
                    if ti == 1:
                        dump("U1_%d" % l, U.ap(), ukeys)
                S.barrier()
                if fused:
                    S.op("pool", lambda e, li=li: e.collective_compute(
                        "AllGather", ALU.bypass, replica_groups=pairs, ins=[yb_in[li][1].ap()], outs=[yb_out[li][1].ap()]),
                        reads=[], writes=["yb_out1"], cc=True)
                S.emit()
            if stop_after == "mixer":
                break

            if not has_tok:
                break
            with ExitStack() as ph:
                h2T = sb("h2T", 8 * NOWN, BF16, stack=ph)
                wo = T(wbig.h, wbig.F, BF16)
                WO = 12288
                bst = sb("bst", 12, stack=ph)
                mv = sb("mv", 4, stack=ph)
                lng = sb("lng", D, stack=ph)
                lnb = sb("lnb", D, stack=ph)
                wst = [sb("wst%d" % i, D, stack=ph) for i in range(4)]
                ph1 = ExitStack()
                yin = [[sb("yin%d_%d" % (i, j), 512, BF16, stack=ph1) for j in range(4)] for i in range(2)]
                yT = sb("yT", 8 * 128, BF16, stack=ph1)
                z = sb("z", D, stack=ph1)
                h2f = sb("h2f", 8 * 128, stack=ph1)
                rt = sb("rt", 160, stack=ph1)

                DMA("sp", lng.ap(), dap(ln1_g, l * D, [[0, 128], [1, D]]), writes=["lng"])
                DMA("sp", lnb.ap(), dap(ln1_b, l * D, [[0, 128], [1, D]]), writes=["lnb"])
                wst_n = [0]
                for kc in range(8):
                    i_ = wst_n[0] % 4
                    wst_n[0] += 1
                    k_w = "wst%d" % i_
                    DMA("sp", wst[i_].ap(), dap(w_out_p, l * D * D + kc * 128 * D, [[D, 128], [1, D]]),
                        writes=[k_w])
                    TT("pool", wo.ap(WO + kc * D, [[1, D]]), wst[i_].ap(), gbc1.ap(), ALU.mult, [k_w, "gbc"], ["wo"])
                def load_expert_w13(e_, slot):
                    base = slot * 12288
                    for wi, wsrc in enumerate((exp_w1, exp_w3)):
                        for kp in range(4):
                            i_ = wst_n[0] % 4
                            wst_n[0] += 1
                            k_w = "wst%d" % i_
                            DMA("sp", wst[i_].ap(0, [[512, 2], [1, 512]]),
                                dap(wsrc, ((li * NE + e_) * D + kp * 256) * DE, [[DE, 128], [128 * DE, 2], [1, DE]]),
                                writes=[k_w])
                            CP("pool", wbig.ap(base + wi * 4096 + kp * 1024, [[1, 1024]]), wst[i_].ap(), [k_w],
                               ["w13_%d" % slot])
                load_expert_w13(0, 0)

                for tj in range(NTILE_TOK):
                    b2 = tj % 2
                    kx = "xres%d" % tj
                    xr = lambda c0, c1, tj=tj: xres.c(tj * D + c0, tj * D + c1)
                    for r in range(2):
                        for hf_ in range(2):
                            DMA("sp", yin[b2][r * 2 + hf_].ap(),
                                (dap(yb_out[li][hf_], (r * NOWN + tj * 128) * 512, [[512, 128], [1, 512]]) if fused else
                                 dap(ygath_d, ((r * SEQ) + hf_ * NOWN + tj * 128) * 512, [[512, 128], [1, 512]])),
                                reads=["yb_out%d" % hf_], writes=["yin%d" % b2])
                    for r in range(2):
                        for fb in range(4):
                            kc = r * 4 + fb
                            bank = kc // 4
                            for hf_ in range(2):
                                MM(pb[bank].c((kc % 4) * 128, (kc % 4) * 128 + 128),
                                   yin[b2][r * 2 + hf_].c(fb * 128, fb * 128 + 128),
                                   (ids0 if hf_ == 0 else ids1).ap(), ["yin%d" % b2, "ids0", "ids1"], ["pb%d" % bank],
                                   start=(hf_ == 0), stop=(hf_ == 1))
                    CP("act", yT.c(0, 512), pb[0].ap(), ["pb0"], ["yT"])
                    CP("act", yT.c(512, 1024), pb[1].ap(), ["pb1"], ["yT"])
                    for n in range(2):
                        for kc in range(8):
                            MM(pb[2 + n].ap(), yT.c(kc * 128, kc * 128 + 128), wo.ap(WO + kc * D + n * 512, [[1, 512]]),
                               ["yT", "wo"], ["pb%d" % (2 + n)], start=(kc == 0), stop=(kc == 7))
                    for n in range(2):
                        STT(z.c(n * 512, n * 512 + 512), xr(n * 512, n * 512 + 512), ALPHA, pb[2 + n].ap(), ALU.mult, ALU.add,
                            [kx, "pb%d" % (2 + n)], ["z"])
                    for n in range(2):
                        S.op("dve", lambda e, o=bst.c(n * 6, n * 6 + 6), i=z.c(n * 512, n * 512 + 512): e.bn_stats(o, i),
                             ["z"], ["bst"])
                    S.op("dve", lambda e, o=mv.c(0, 2), i=bst.ap(): e.bn_aggr(o, i), ["bst"], ["mv"])
                    TS("dve", mv.c(2, 3), mv.c(1, 2), LN_EPS, None, ALU.add, None, ["mv"], ["mv_r"])
                    ACT(mv.c(2, 3), mv.c(2, 3), AF.Sqrt, ["mv_r"], ["mv_r"])
                    RECIP(mv.c(2, 3), mv.c(2, 3), ["mv_r"], ["mv_r"])
                    TS("dve", z.ap(), z.ap(), mv.c(0, 1), mv.c(2, 3), ALU.subtract, ALU.mult, ["z", "mv", "mv_r"], ["z"])
                    TT("pool", z.ap(), z.ap(), lng.ap(), ALU.mult, ["z", "lng"], ["z"])
                    TT("dve", xr(0, D), z.ap(), lnb.ap(), ALU.add, ["z", "lnb"], [kx])
                    if tj == 0:
                        dump("x1_%d" % l, xr(0, D), [kx])
                    for kc in range(8):
                        bank = 4 + kc // 4
                        TR(pb[bank].c((kc % 4) * 128, (kc % 4) * 128 + 128), xr(kc * 128, kc * 128 + 128), ident,
                           [kx, "cst"], ["pb%d" % bank])
                    for kc in range(8):
                        bank = 4 + kc // 4
                        src = pb[bank].c((kc % 4) * 128, (kc % 4) * 128 + 128)
                        dst = h2f.c(kc * 128, kc * 128 + 128)
                        if kc % 2 == 0:
                            ACT(dst, src, AF.Identity, ["pb%d" % bank, "modT"], ["h2f"],
                                scale=modT.c(16 + kc, 17 + kc), bias=modT.c(24 + kc, 25 + kc))
                        else:
                            TS("dve", dst, src, modT.c(16 + kc, 17 + kc), modT.c(24 + kc, 25 + kc), ALU.mult, ALU.add,
                               ["pb%d" % bank, "modT"], ["h2f"])
                    CP("pool", h2T.ap(tj * 128, [[NOWN, 8], [1, 128]]), h2f.ap(0, [[128, 8], [1, 128]]), ["h2f"], ["h2T"])
                    for kc in range(8):
                        MM(pb[6].c(0, NE), h2f.c(kc * 128, kc * 128 + 128), rwf.c(kc * NE, kc * NE + NE), ["h2f", "rwf"],
                           ["pb6"], start=(kc == 0), stop=(kc == 7))
                    R = lambda a, b_: rt.c(a, b_)
                    R3 = lambda a: rt.ap(a, [[4, 4], [1, 4]])
                    BIG = 1.0e4
                    CP("act", R(128, 144), pb[6].c(0, NE), ["pb6"], ["rt_lg"])
                    RED(R(0, 1), R(128, 144), ALU.max, ["rt_lg"], ["rt_m"])
                    TS("dve", R(1, 2), R(0, 1), -1.0, None, ALU.mult, None, ["rt_m"], ["rt_nm"])
                    ACT(R(16, 32), R(128, 144), AF.Exp, ["rt_lg", "rt_nm"], ["rt_e"], bias=R(1, 2))
                    RED(R(2, 3), R(16, 32), ALU.add, ["rt_e"], ["rt_s"])
                    RECIP(R(2, 3), R(2, 3), ["rt_s"], ["rt_s"])
                    TS("dve", R(16, 32), R(16, 32), R(2, 3), None, ALU.mult, None, ["rt_e", "rt_s"], ["rt_p"])
                    TT("dve", R(32, 48), R(16, 32), rbb.ap(), ALU.add, ["rt_p", "rbb"], ["rt_sel"])
                    RED(R(4, 8), R3(32), ALU.max, ["rt_sel"], ["rt_g1"])
                    TT("dve", R3(48), R3(32), rt.ap(4, [[1, 4], [0, 4]]), ALU.is_equal, ["rt_sel", "rt_g1"], ["rt_eq"])
                    STT(R(48, 64), R(48, 64), -BIG, R(32, 48), ALU.mult, ALU.add, ["rt_eq", "rt_sel"], ["rt_v2"])
                    RED(R(8, 12), R3(48), ALU.max, ["rt_v2"], ["rt_g2"])
                    TT("dve", R(4, 8), R(4, 8), R(8, 12), ALU.add, ["rt_g1", "rt_g2"], ["rt_gs"])
                    RED(R(3, 4), R(4, 8), ALU.max, ["rt_gs"], ["rt_gm"])
                    TS("dve", R(8, 12), R(4, 8), R(3, 4), None, ALU.is_equal, None, ["rt_gs", "rt_gm"], ["rt_ing"])
                    TS("dve", R(8, 12), R(8, 12), -1.0, BIG, ALU.add, ALU.mult, ["rt_ing"], ["rt_pen"])
                    TT("dve", R3(64), R3(32), rt.ap(8, [[1, 4], [0, 4]]), ALU.add, ["rt_sel", "rt_pen"], ["rt_msk"])
                    RED(R(12, 13), R(64, 80), ALU.max, ["rt_msk"], ["rt_t1"])
                    TS("dve", R(80, 96), R(64, 80), R(12, 13), None, ALU.is_equal, None, ["rt_msk", "rt_t1"], ["rt_oh1"])
                    STT(R(96, 112), R(80, 96), -BIG, R(64, 80), ALU.mult, ALU.add, ["rt_oh1", "rt_msk"], ["rt_m2"])
                    RED(R(13, 14), R(96, 112), ALU.max, ["rt_m2"], ["rt_t2"])
                    TS("dve", R(112, 128), R(96, 112), R(13, 14), None, ALU.is_equal, None, ["rt_m2", "rt_t2"], ["rt_oh2"])
                    TT("dve", R(80, 96), R(80, 96), R(112, 128), ALU.add, ["rt_oh1", "rt_oh2"], ["rt_sm"])
                    TT("dve", R(80, 96), R(80, 96), R(16, 32), ALU.mult, ["rt_sm", "rt_p"], ["rt_pw"])
                    RED(R(14, 15), R(80, 96), ALU.add, ["rt_pw"], ["rt_ws"])
                    RECIP(R(14, 15), R(14, 15), ["rt_ws"], ["rt_ws"])
                    TS("dve", gates.c(tj * NE, tj * NE + NE), R(80, 96), R(14, 15), None, ALU.mult, None,
                       ["rt_pw", "rt_ws"], ["gates"])
                    if tj == 0:
                        dump("gates%d" % l, gates.c(0, NE), ["gates"])
                S.barrier()
                S.emit()
                ph1.close()
                if stop_after == "tok1":
                    break

                with ExitStack() as ph2:
                    sa = [sb("sa%d" % i, 512, stack=ph2) for i in range(2)]
                    sbb = [sb("sbb%d" % i, 512, stack=ph2) for i in range(2)]
                    yv = [sb("yv%d" % i, 512, stack=ph2) for i in range(2)]
                    gT = [sb("gT%d" % i, 4 * 512, BF16, stack=ph2) for i in range(2)]
                    DMA("sp", lng.ap(), dap(ln2_g, l * D, [[0, 128], [1, D]]), writes=["lng"])
                    DMA("sp", lnb.ap(), dap(ln2_b, l * D, [[0, 128], [1, D]]), writes=["lnb"])

                    def load_w2(e_, slot):
                        for fc in range(4):
                            i_ = wst_n[0] % 4
                            wst_n[0] += 1
                            k_w = "wst%d" % i_
                            DMA("sp", wst[i_].ap(),
                                dap(exp_w2, ((li * NE + e_) * DE + fc * 128) * D, [[D, 128], [1, D]]), writes=[k_w])
                            TT("pool", wbig.ap(slot * 12288 + 8192 + fc * D, [[1, D]]), wst[i_].ap(), gbc2.ap(), ALU.mult,
                               [k_w, "gbc"], ["w2b%d" % slot])
                    load_w2(0, 0)
                    cnt = [0]

                    def emit_w13(e_, tg):
                        slot = e_ % 2
                        base = slot * 12288
                        g_ = gT[tg % 2]
                        k_g = "gT%d" % (tg % 2)
                        for fb in range(4):
                            ba, bb = cnt[0] % 2, 2 + cnt[0] % 2
                            cnt[0] += 1
                            for kc in range(8):
                                MM(pb[ba].ap(), wbig.ap(base + kc * 512 + fb * 128, [[1, 128]]),
                                   h2T.ap(kc * NOWN + tg * 512, [[1, 512]]), ["w13_%d" % slot, "h2T"], ["pb%d" % ba],
                                   start=(kc == 0), stop=(kc == 7))
                            for kc in range(8):
                                MM(pb[bb].ap(), wbig.ap(base + 4096 + kc * 512 + fb * 128, [[1, 128]]),
                                   h2T.ap(kc * NOWN + tg * 512, [[1, 512]]), ["w13_%d" % slot, "h2T"], ["pb%d" % bb],
                                   start=(kc == 0), stop=(kc == 7))
                            ACT(sa[ba].ap(), pb[ba].ap(), AF.Silu, ["pb%d" % ba], ["sa%d" % ba])
                            CP("act", sbb[ba].ap(), pb[bb].ap(), ["pb%d" % bb], ["sbb%d" % ba])
                            TT("dve", g_.c(fb * 512, fb * 512 + 512), sa[ba].ap(), sbb[ba].ap(), ALU.mult,
                               ["sa%d" % ba, "sbb%d" % ba], [k_g])

                    def emit_w2(e_, tg):
                        slot = e_ % 2
                        base = slot * 12288
                        g_ = gT[tg % 2]
                        k_g = "gT%d" % (tg % 2)
                        for tt in range(4):
                            tile_ = tg * 4 + tt
                            kx = "xres%d" % tile_
                            for n in range(2):
                                by = 4 + (tt * 2 + n) % 4
                                for fc in range(4):
                                    MM(pb[by].ap(), g_.c(fc * 512 + tt * 128, fc * 512 + tt * 128 + 128),
                                       wbig.ap(base + 8192 + fc * D + n * 512, [[1, 512]]), [k_g, "w2b%d" % slot],
                                       ["pb%d" % by], start=(fc == 0), stop=(fc == 3))
                                xv = xres.c(tile_ * D + n * 512, tile_ * D + n * 512 + 512)
                                yi = (tt * 2 + n) % 2
                                ACT(yv[yi].ap(), pb[by].ap(), AF.Identity, ["pb%d" % by, "gates", "cst"], ["yv%d" % yi],
                                    scale=gates.c(tile_ * NE + e_, tile_ * NE + e_ + 1), bias=cst.c(C_CM + 4, C_CM + 5))
                                TT("dve", xv, xv, yv[yi].ap(), ALU.add, ["yv%d" % yi, kx], [kx])

                    steps = [(e_, tg) for e_ in range(NE) for tg in range(4)]
                    for k, (e_, tg) in enumerate(steps):
                        emit_w13(e_, tg)
                        if k > 0:
                            emit_w2(*steps[k - 1])
                        if tg == 0 and e_ + 1 < NE:
                            load_expert_w13(e_ + 1, 1 - e_ % 2)
                            load_w2(e_ + 1, 1 - e_ % 2)
                    emit_w2(*steps[-1])
                    for tj in range(NTILE_TOK):
                        kx = "xres%d" % tj
                        xr = lambda c0, c1, tj=tj: xres.c(tj * D + c0, tj * D + c1)
                        if tj == 0:
                            dump("z2_%d" % l, xr(0, D), [kx])
                        for n in range(2):
                            S.op("dve", lambda e, o=bst.c(n * 6, n * 6 + 6), i=xr(n * 512, n * 512 + 512): e.bn_stats(o, i),
                                 [kx], ["bst"])
                        S.op("dve", lambda e, o=mv.c(0, 2), i=bst.ap(): e.bn_aggr(o, i), ["bst"], ["mv"])
                        TS("dve", mv.c(2, 3), mv.c(1, 2), LN_EPS / (ALPHA * ALPHA), None, ALU.add, None, ["mv"], ["mv_r"])
                        ACT(mv.c(2, 3), mv.c(2, 3), AF.Sqrt, ["mv_r"], ["mv_r"])
                        RECIP(mv.c(2, 3), mv.c(2, 3), ["mv_r"], ["mv_r"])
                        TS("dve", xr(0, D), xr(0, D), mv.c(0, 1), mv.c(2, 3), ALU.subtract, ALU.mult, [kx, "mv", "mv_r"], [kx])
                        TT("pool", xr(0, D), xr(0, D), lng.ap(), ALU.mult, [kx, "lng"], [kx])
                        TT("dve", xr(0, D), xr(0, D), lnb.ap(), ALU.add, [kx, "lnb"], [kx])
                        xdst = dap(out_d, tj * 128 * D, [[D, 128], [1, D]]) if last else \
                            dap(xg_in[tj // 4], (tj % 4) * 128 * D, [[D, 128], [1, D]])
                        DMA("sp", xdst, xr(0, D), reads=[kx], writes=["xg_in"])
                    S.barrier()
                    if not last:
                        for q in range(4):
                            S.op("pool", lambda e, q=q: e.collective_compute(
                                "AllGather", ALU.bypass, replica_groups=pairs, ins=[xg_in[q].ap()], outs=[xg_out[q].ap()]),
                                reads=["xg_in"], writes=["xg_out"], cc=True)
                    S.emit()
        print("bass program: %d scheduled ops" % S.n_inst)
    return nc


W = 256
_SPLITS = (W, W, W, W, W, W, W, W, 4, 4, W, W, W, W, 16, W, W, W, W)
_OFFS = np.concatenate([[0], np.cumsum(_SPLITS)]).astype(int)
_WIDE = [0, 1, 2, 3, 4, 5, 6, 7, 10, 11, 12, 13, 15, 16, 17, 18]


def _consts():
    c = np.zeros((128, NCONST), np.float32)
    i = np.arange(128)
    c[i, C_ID + i] = 1.0
    jj, ii = np.meshgrid(i, i, indexing="ij")
    same = (jj // 32) == (ii // 32)
    lt = (same & (jj <= ii)).astype(np.float32)
    c[:, C_LT:C_LT + 128] = lt
    c[:, C_MASK:C_MASK + 128] = lt
    for cc in range(4):
        c[cc * 32:(cc + 1) * 32, C_SEL + cc] = 1.0
    for s in (1, 2, 3):
        c[:, C_SH + (s - 1) * 128:C_SH + s * 128] = (jj == ii - s)
        c[:, C_SP + (s - 1) * 128:C_SP + s * 128] = (jj == 128 + ii - s)
    c[:, C_CM:C_CM + 4] = 1.0 / 64.0
    c[:64, C_HM] = 1.0
    c[64:, C_HM + 1] = 1.0
    return c


def _rot_table():
    inv = (10000.0 ** (-np.arange(0, 64, 2, dtype=np.float32) / np.float32(64))).astype(np.float32)
    ang = np.arange(SEQ, dtype=np.float32)[:, None] * inv[None, :]
    return np.concatenate([np.cos(ang), np.sin(ang)], axis=1).astype(np.float32)


def make_in_maps(inp, layer, part, x_cur, ys=None, n_cores=8, layers=None):
    f = lambda a: np.ascontiguousarray(np.asarray(a, dtype=np.float32))
    x, c = f(x_cur), f(inp["c"])
    w_in = f(inp["w_in"])
    consts = _consts()
    rot = _rot_table()
    ls = [layer] if layers is None else list(layers)
    shared = {
        "ada_w": np.ascontiguousarray(f(inp["ada_w"])[ls]),
        "ada_bT": np.ascontiguousarray(f(inp["ada_b"]).reshape(DEPTH, 48, 128).transpose(0, 2, 1)),
        "ada_bg": np.ascontiguousarray(np.stack([f(inp["ada_b"])[:, 2048:3072], f(inp["ada_b"])[:, 5120:6144]], axis=1)),
        "ln1_g": f(inp["ln1_g"]), "ln1_b": f(inp["ln1_b"]), "ln2_g": f(inp["ln2_g"]), "ln2_b": f(inp["ln2_b"]),
        "router_w": f(inp["router_w"]), "router_b": f(inp["router_b"]).reshape(1, NE),
        "consts": consts, "rot": rot,
    }
    if part in ("tok", "fused"):
        shared["exp_w1"] = np.ascontiguousarray(f(inp["exp_w1"])[ls])
        shared["exp_w3"] = np.ascontiguousarray(f(inp["exp_w3"])[ls])
        shared["exp_w2"] = np.ascontiguousarray(f(inp["exp_w2"])[ls])
    per_s = []
    for s in range(2):
        hs = slice(2 * s * 64, (2 * s + 2) * 64)
        cols = []
        for q in _WIDE:
            cols.append(np.arange(_OFFS[q] + 2 * s * 64, _OFFS[q] + (2 * s + 2) * 64))
        cols.append(np.arange(_OFFS[8] + 2 * s, _OFFS[8] + 2 * s + 2))
        cols.append(np.arange(_OFFS[9] + 2 * s, _OFFS[9] + 2 * s + 2))
        cols.append(np.arange(_OFFS[14], _OFFS[14] + 16))
        cols = np.concatenate(cols)
        conv = f(inp["mlstm_conv"])
        gb = f(inp["mlstm_gate_b"])
        perm = np.concatenate([np.arange(m * 256 + r * 128, m * 256 + r * 128 + 128) for r in range(2) for m in range(4)])
        lg = np.zeros((128, 128), np.float32)
        for hl in range(2):
            lg[:, hl * 64:(hl + 1) * 64] = np.float32(np.log1p(-(2.0 ** (-5.0 - (2 * s + hl)))))
        per_s.append({
            "w_in_c": np.ascontiguousarray(w_in[:, :, cols]),
            "conv_c": np.ascontiguousarray(np.concatenate([conv[:, :, hs], conv[:, :, 256:][:, :, hs]], axis=2)),
            "gateb_c": np.ascontiguousarray(np.concatenate([gb[:, 2 * s:2 * s + 2], gb[:, 4 + 2 * s:4 + 2 * s + 2]], axis=1)),
            "glaw2_c": np.ascontiguousarray(np.concatenate([f(inp["gla_w2"])[:, :, hs], f(inp["gla_b2"])[:, None, hs]], axis=1)),
            "hlb_c": np.ascontiguousarray(f(inp["hgrn_lb"])[:, hs]),
            "gain_c": np.ascontiguousarray(np.concatenate([f(inp["ret_norm"])[:, hs], f(inp["mlstm_norm"])[:, hs],
                                                           f(inp["gla_norm"])[:, hs], f(inp["hgrn_norm"])[:, hs]], axis=1)),
            "w_out_p": np.ascontiguousarray(f(inp["w_out"])[:, perm, :]),
            "lgam": lg,
            "sel": np.ascontiguousarray(np.tile(np.array([[1.0 - s, float(s)]], np.float32), (128, 1))),
        })
    maps = []
    for core in range(n_cores):
        b, s = core // 2, core % 2
        m = dict(shared)
        m.update(per_s[s])
        if part in ("mix", "fused"):
            m["xfull"] = np.ascontiguousarray(x[b])
        if part == "tok":
            m["ygath"] = np.ascontiguousarray(np.concatenate([np.asarray(ys[2 * b]), np.asarray(ys[2 * b + 1])], axis=0))
        m["xown"] = np.ascontiguousarray(x[b, s * NOWN:(s + 1) * NOWN])
        m["cT"] = np.ascontiguousarray(c[b].reshape(8, 128).T)
        maps.append(m)
    return maps


def run_layer(inputs, l, x_cur, n_cores=8, dbg=None):
    cores = list(range(n_cores))
    nc = build(layer=l, part="mix", n_cores=n_cores)
    res = run_bass_kernel_spmd(nc, make_in_maps(inputs, l, "mix", x_cur, n_cores=n_cores), core_ids=cores)
    ys = [res.results[c]["yout"] for c in cores]
    nc2 = build(layer=l, part="tok", n_cores=n_cores, dbg=dbg)
    res2 = run_bass_kernel_spmd(nc2, make_in_maps(inputs, l, "tok", x_cur, ys, n_cores=n_cores), core_ids=cores)
    x_new = np.array(x_cur, dtype=np.float32, copy=True)
    for core in cores:
        b, s = core // 2, core % 2
        x_new[b, s * NOWN:(s + 1) * NOWN] = res2.results[core]["out"]
    return x_new, res2


def run_fused(inputs, layers, x_cur, n_cores=8, dbg=None):
    cores = list(range(n_cores))
    nc = build(part="fused", layers=layers, n_cores=n_cores, dbg=dbg)
    res = run_bass_kernel_spmd(nc, make_in_maps(inputs, layers[0], "fused", x_cur, n_cores=n_cores, layers=layers),
                               core_ids=cores)
    x_new = np.array(x_cur, dtype=np.float32, copy=True)
    for core in cores:
        b, s = core // 2, core % 2
        x_new[b, s * NOWN:(s + 1) * NOWN] = res.results[core]["out"]
    return x_new, res


def kernel(**inputs):
    x_cur = np.asarray(inputs["x"], dtype=np.float32)
    x_cur, _ = run_fused(inputs, list(range(DEPTH)), x_cur)
    return x_cur
```
